# Optimizing a Trainium2 kernel written in Bass

```python
import math
import jax, jax.numpy as jnp
from jax import lax
import numpy as np

D_MODEL = 1024
BATCH = 16
SEQ = 4096
DEPTH = 1

HEAD_DIM = 64
N_HEADS_A = 8
N_HEADS_B = 8
N_HEADS = N_HEADS_A + N_HEADS_B
D_A = N_HEADS_A * HEAD_DIM
D_B = N_HEADS_B * HEAD_DIM
D_MIX = D_A + D_B
N_IDX_HEADS = 4
IDX_DIM = 64
TOPK_MAX = 256
DIL_PATTERNS = ((128, 1), (512, 4), (2048, 16))
BLOCK = 128
N_BUCKETS = 32
MAX_DISTANCE = 128
EPS = 1e-6
SPLITS = (D_A, D_A, D_A, D_A, N_IDX_HEADS * IDX_DIM, IDX_DIM, N_IDX_HEADS, D_B, D_B, D_B, D_B)
D_IN = sum(SPLITS)

kernel_name = "hybrid_dsa_dilated_parallel_heads"


def rms_norm(x, g):
    xf = x.astype(jnp.float32)
    y = xf * lax.rsqrt(jnp.mean(xf * xf, axis=-1, keepdims=True) + EPS) * g.astype(jnp.float32)
    return y.astype(x.dtype)


def rel_bucket(dist):
    max_exact = N_BUCKETS // 2
    d = jnp.maximum(dist, 0)
    df = jnp.maximum(d, 1).astype(jnp.float32)
    large = max_exact + (jnp.log(df / max_exact) / math.log(MAX_DISTANCE / max_exact)
                         * (N_BUCKETS - max_exact)).astype(jnp.int32)
    large = jnp.minimum(large, N_BUCKETS - 1)
    return jnp.where(d < max_exact, d, large)


def dsa_attention(q, k, v, q_idx, k_idx, w_idx, bias_table):
    b, s, h, dh = q.shape
    topk = min(TOPK_MAX, s // 4)
    nblk = s // BLOCK
    scale = HEAD_DIM ** -0.5
    idx_scale = (N_IDX_HEADS * IDX_DIM) ** -0.5
    key_pos = jnp.arange(s)

    def to_blocks(a):
        return jnp.moveaxis(a.reshape(b, nblk, BLOCK, *a.shape[2:]), 1, 0)

    def block_fn(args):
        qb, qib, wb, start = args
        q_pos = start + jnp.arange(BLOCK)
        rel = jnp.einsum('bqhd,bsd->bqhs', qib, k_idx)
        score = jnp.einsum('bqhs,bqh->bqs', jax.nn.relu(rel).astype(jnp.float32),
                           wb.astype(jnp.float32)) * idx_scale
        causal = key_pos[None, :] <= q_pos[:, None]
        score = jnp.where(causal[None], score, -jnp.inf)
        top_val, top_idx = lax.top_k(score, topk)
        valid = jnp.isfinite(top_val)
        k_sel = jax.vmap(lambda kk, ii: kk[ii])(k, top_idx)
        v_sel = jax.vmap(lambda vv, ii: vv[ii])(v, top_idx)
        logits = jnp.einsum('bqhd,bqkhd->bqhk', qb, k_sel).astype(jnp.float32) * scale
        bucket = rel_bucket(q_pos[None, :, None] - top_idx)
        bias = jnp.moveaxis(bias_table[bucket], -1, -2).astype(jnp.float32)
        logits = jnp.where(valid[:, :, None, :], logits + bias, -jnp.inf)
        p = jax.nn.softmax(logits, axis=-1)
        return jnp.einsum('bqhk,bqkhd->bqhd', p.astype(v.dtype), v_sel)

    starts = jnp.arange(nblk) * BLOCK
    out = lax.map(block_fn, (to_blocks(q), to_blocks(q_idx), to_blocks(w_idx), starts))
    return jnp.moveaxis(out, 0, 1).reshape(b, s, h, dh)


def dilated_pattern(q, k, v, window, dilation, bias_table):
    b, s, h, dh = q.shape
    seg = dilation * BLOCK
    p_len = -(-s // seg) * seg
    pad = p_len - s
    n_sub = p_len // dilation
    nb = n_sub // BLOCK
    steps = window // dilation
    scale = HEAD_DIM ** -0.5

    def to_segments(a):
        a = jnp.pad(a, ((0, 0), (0, pad), (0, 0), (0, 0)))
        a = a.reshape(b, n_sub, dilation, h, dh).transpose(2, 0, 1, 3, 4)
        return a.reshape(dilation, b, nb, BLOCK, h, dh)

    def band(a):
        prev = jnp.pad(a, ((0, 0), (0, 0), (1, 0), (0, 0), (0, 0), (0, 0)))[:, :, :-1]
        return jnp.concatenate([prev, a], axis=3)

    def seg_major(a):
        return jnp.moveaxis(a, 2, 1).reshape(dilation * nb, b, *a.shape[3:])

    qs, ks, vs = to_segments(q), to_segments(k), to_segments(v)
    qs, kb, vb = seg_major(qs), seg_major(band(ks)), seg_major(band(vs))
    blk_ids = jnp.arange(dilation * nb) % nb

    a_idx = jnp.arange(BLOCK)[:, None]
    c_idx = jnp.arange(2 * BLOCK)[None, :]
    diff = a_idx - c_idx + BLOCK
    band_mask = (diff >= 0) & (diff <= steps)
    bias = jnp.transpose(bias_table[rel_bucket(diff * dilation)], (2, 0, 1)).astype(jnp.float32)

    def seg_fn(args):
        qq, kk, vv, blk = args
        key_ok = (blk * BLOCK - BLOCK + jnp.arange(2 * BLOCK)) >= 0
        mask = band_mask & key_ok[None, :]
        logits = jnp.einsum('bqhd,bkhd->bhqk', qq, kk).astype(jnp.float32) * scale + bias
        logits = jnp.where(mask, logits, -jnp.inf)
        m = jnp.max(logits, axis=-1)
        pexp = jnp.exp(logits - m[..., None])
        den = jnp.sum(pexp, axis=-1)
        num = jnp.einsum('bhqk,bkhd->bqhd', pexp, vv.astype(jnp.float32))
        return jnp.swapaxes(m, 1, 2), jnp.swapaxes(den, 1, 2), num

    m, den, num = lax.map(seg_fn, (qs, kb, vb, blk_ids))

    def unseg(a):
        rest = a.shape[3:]
        a = a.reshape(dilation, nb, b, BLOCK, *rest)
        a = jnp.moveaxis(jnp.moveaxis(a, 0, 3), 0, 1)
        return a.reshape(b, p_len, *rest)[:, :s]

    return unseg(m), unseg(den), unseg(num)


def dilated_attention(q, k, v, bias_table):
    stats = [dilated_pattern(q, k, v, w, d, bias_table) for (w, d) in DIL_PATTERNS]
    m_all = jnp.stack([st[0] for st in stats])
    wts = jnp.exp(m_all - jnp.max(m_all, axis=0))
    den = sum(wts[i] * stats[i][1] for i in range(len(stats)))
    num = sum(wts[i][..., None] * stats[i][2] for i in range(len(stats)))
    return (num / den[..., None]).astype(q.dtype)


def setup_inputs(seed: int = 0) -> dict:
    key = jax.random.key(seed)
    ks = jax.random.split(key, 9)
    x = jax.random.normal(ks[0], (BATCH, SEQ, D_MODEL), jnp.float32)
    norm_gain = 1.0 + 0.02 * jax.random.normal(ks[1], (DEPTH, D_MODEL), jnp.float32)
    w_in = jax.random.normal(ks[2], (DEPTH, D_MODEL, D_IN), jnp.float32) * D_MODEL ** -0.5
    w_out = jax.random.normal(ks[3], (DEPTH, D_MIX, D_MODEL), jnp.float32) * D_MIX ** -0.5
    rel_bias = 0.1 * jax.random.normal(ks[4], (N_BUCKETS, N_HEADS), jnp.float32)
    q_norm_a = 1.0 + 0.02 * jax.random.normal(ks[5], (DEPTH, HEAD_DIM), jnp.float32)
    k_norm_a = 1.0 + 0.02 * jax.random.normal(ks[6], (DEPTH, HEAD_DIM), jnp.float32)
    q_norm_b = 1.0 + 0.02 * jax.random.normal(ks[7], (DEPTH, HEAD_DIM), jnp.float32)
    k_norm_b = 1.0 + 0.02 * jax.random.normal(ks[8], (DEPTH, HEAD_DIM), jnp.float32)
    return {"x": x, "norm_gain": norm_gain, "w_in": w_in, "w_out": w_out, "rel_bias": rel_bias,
            "q_norm_a": q_norm_a, "k_norm_a": k_norm_a, "q_norm_b": q_norm_b, "k_norm_b": k_norm_b}


def reference(x, norm_gain, w_in, w_out, rel_bias, q_norm_a, k_norm_a, q_norm_b, k_norm_b):
    b, s, _ = x.shape
    split_points = np.cumsum(SPLITS)[:-1].tolist()
    bias_a = rel_bias[:, :N_HEADS_A]
    bias_b = rel_bias[:, N_HEADS_A:]
    for layer in range(DEPTH):
        xn = rms_norm(x, norm_gain[layer])
        proj = jnp.einsum('bsd,de->bse', xn, w_in[layer])
        qa, ka, va, za, qi, ki, wi, qb, kb, vb, zb = jnp.split(proj, split_points, axis=-1)
        qa = rms_norm(qa.reshape(b, s, N_HEADS_A, HEAD_DIM), q_norm_a[layer])
        ka = rms_norm(ka.reshape(b, s, N_HEADS_A, HEAD_DIM), k_norm_a[layer])
        va = va.reshape(b, s, N_HEADS_A, HEAD_DIM)
        qi = qi.reshape(b, s, N_IDX_HEADS, IDX_DIM)
        qb = rms_norm(qb.reshape(b, s, N_HEADS_B, HEAD_DIM), q_norm_b[layer])
        kb = rms_norm(kb.reshape(b, s, N_HEADS_B, HEAD_DIM), k_norm_b[layer])
        vb = vb.reshape(b, s, N_HEADS_B, HEAD_DIM)
        a_out = dsa_attention(qa, ka, va, qi, ki, wi, bias_a)
        b_out = dilated_attention(qb, kb, vb, bias_b)
        mixed = jnp.concatenate([a_out.reshape(b, s, D_A) * jax.nn.silu(za),
                                 b_out.reshape(b, s, D_B) * jax.nn.silu(zb)], axis=-1)
        x = x + jnp.einsum('bse,ed->bsd', mixed, w_out[layer])
    return x
```

```python
import math
import contextlib
import numpy as np
import ml_dtypes
import concourse.bass as bass
import concourse.mybir as mybir
from concourse.bass_utils import run_bass_kernel_spmd

F32 = mybir.dt.float32
BF16 = mybir.dt.bfloat16
I32 = mybir.dt.int32
ALU = mybir.AluOpType
ACTF = mybir.ActivationFunctionType

NDMA_SEM = 12
D_MODEL = 1024
D_IN = 4420
EPS = 1e-6
C_QA, C_KA, C_VA, C_ZA, C_QI, C_KI, C_WI, C_QB, C_KB, C_VB, C_ZB = (
    0, 512, 1024, 1536, 2048, 2304, 2368, 2372, 2884, 3396, 3908)
DILS = (1, 4, 16)
NEGBIG = -30000.0
WSCALE = 2.0 ** -12


class T:
    __slots__ = ("t", "writers", "readers", "name", "full")

    def __init__(self, t, name=""):
        self.t = t
        self.writers = []
        self.readers = []
        self.name = name
        self.full = None

    def __getitem__(self, k):
        return self.t[k]


class Prog:
    ENGS = ("pe", "act", "dve", "pool", "sp")

    def __init__(self, nc, st):
        self.nc = nc
        self.ops = {e: [] for e in self.ENGS}
        self.count = {e: 0 for e in self.ENGS}
        self.seen = {e: {} for e in self.ENGS}
        self.dma_n = {"sp": 0, "pool": 0}
        self.dma_last = {}
        self.pending = {e: [] for e in self.ENGS}
        self.n_instr = 0
        self.sems = {}
        for e in ("pe", "act", "dve", "pool"):
            self.sems[e] = st.enter_context(nc.semaphore("s_" + e))
        for q in ("sp", "pool"):
            for j in range(NDMA_SEM):
                self.sems[("dma", q, j)] = st.enter_context(nc.semaphore(f"d_{q}{j}"))

    def _deps(self, eng, reads, writes, pwrites):
        deps = {}

        def add(tok):
            k, v = tok
            if deps.get(k, 0) < v:
                deps[k] = v
        for t in reads:
            for w in t.writers:
                add(w)
        for t in writes:
            for w in t.writers:
                add(w)
            for r in t.readers:
                add(r)
        for t in pwrites:
            for r in t.readers:
                add(r)
            if t.full is not None:
                add(t.full)
        if self.pending[eng]:
            for tok in self.pending[eng]:
                add(tok)
            self.pending[eng] = []
        return deps

    def _commit(self, tok, reads, writes, pwrites):
        for t in reads:
            t.readers.append(tok)
        for t in writes:
            t.writers = [tok]
            t.readers = []
            t.full = tok
        for t in pwrites:
            t.writers.append(tok)

    def _waits(self, eng, deps):
        ws = []
        seen = self.seen[eng]
        for k, v in deps.items():
            if k == "pe" and eng == "pe":
                continue
            if seen.get(k, 0) < v:
                seen[k] = v
                ws.append((k, v))
        return ws

    def op(self, eng, fn, reads=(), writes=(), pwrites=()):
        deps = self._deps(eng, reads, writes, pwrites)
        ws = self._waits(eng, deps)
        self.count[eng] += 1
        tok = (eng, self.count[eng])
        self.ops[eng].append((1, fn, ws, tok))
        self._commit(tok, reads, writes, pwrites)
        self.n_instr += 1
        return tok

    def dma(self, q, fn, reads=(), writes=(), pwrites=()):
        deps = self._deps(q, reads, writes, pwrites)
        n = self.dma_n[q]
        self.dma_n[q] = n + 1
        key = ("dma", q, n % NDMA_SEM)
        val = 16 * (n // NDMA_SEM + 1)
        if n >= NDMA_SEM and deps.get(key, 0) < val - 16:
            deps[key] = val - 16
        ws = self._waits(q, deps)
        tok = (key, val)
        self.dma_last[key] = val
        self.ops[q].append((16, fn, ws, tok))
        self._commit(tok, reads, writes, pwrites)
        self.n_instr += 1
        return tok

    def barrier(self):
        toks = [(e, self.count[e]) for e in ("pe", "act", "dve", "pool") if self.count[e]]
        toks += list(self.dma_last.items())
        for e in self.ENGS:
            self.pending[e] = list(toks)

    def flush(self, final_tokens=None):
        nc = self.nc
        sems = self.sems
        ops = self.ops
        self.ops = {e: [] for e in self.ENGS}

        def run(name, eng):
            for inc, fn, ws, tok in ops[name]:
                for k, v in ws:
                    eng.wait_ge(sems[k], v)
                fn(eng).then_inc(sems[tok[0]], inc)
            if name == "sp" and final_tokens:
                for k, v in final_tokens.items():
                    eng.wait_ge(sems[k], v)

        with nc.Block() as block:
            @block.tensor
            def _(e):
                run("pe", e)

            @block.scalar
            def _(e):
                run("act", e)

            @block.vector
            def _(e):
                run("dve", e)

            @block.gpsimd
            def _(e):
                run("pool", e)

            @block.sync
            def _(e):
                run("sp", e)


def _mm(o, l, r, start, stop):
    return lambda e: e.matmul(o, lhsT=l, rhs=r, start=start, stop=stop)


def _act(o, i, func, bias=None, scale=1.0, accum=None):
    kw = {}
    if bias is not None:
        kw["bias"] = bias
    if accum is not None:
        kw["accum_out"] = accum
    return lambda e: e.activation(out=o, in_=i, func=func, scale=scale, **kw)


def _ts(o, i, s1, s2, op0, op1=None, accum=None):
    kw = {}
    if op1 is not None:
        kw["op1"] = op1
    if accum is not None:
        kw["accum_out"] = accum
    return lambda e: e.tensor_scalar(out=o, in0=i, scalar1=s1, scalar2=s2, op0=op0, **kw)


def _tt(o, a, b, op):
    return lambda e: e.tensor_tensor(out=o, in0=a, in1=b, op=op)


def _stt(o, a, s, b, op0, op1):
    return lambda e: e.scalar_tensor_tensor(out=o, in0=a, scalar=s, in1=b, op0=op0, op1=op1)


def _cp(o, i):
    return lambda e: e.tensor_copy(out=o, in_=i)


def _acp(o, i):
    return lambda e: e.activation(out=o, in_=i, func=ACTF.Copy)


def _dma(o, i):
    return lambda e: e.dma_start(out=o, in_=i)


def _tr(o, i, ident):
    return lambda e: e.transpose(o, i, ident)


def build(S, NB):
    TOPK = min(256, S // 4)
    KTH = float(TOPK) - 0.5
    NT = S // 128
    NG = S // 512
    nc = bass.Bass("TRN2", target_bir_lowering=False)

    def din(name, shape, dt):
        return nc.dram_tensor(name, shape, dt, kind="ExternalInput").ap()

    def dscr(name, shape, dt):
        return nc.dram_tensor(name, shape, dt, kind="Internal")

    x_d = din("x", [NB, S, D_MODEL], F32)
    win_d = din("w_in", [D_MODEL, D_IN], F32)
    wout_d = din("w_out", [D_MODEL, D_MODEL], F32)
    gain8_d = din("gain8", [128, 8], F32)
    relb_d = din("relb", [32, 16], F32)
    qkg_d = din("qkg", [128, 4], F32)
    ident_d = din("ident", [128, 128], BF16)
    bones_d = din("bones", [128, 128], BF16)
    caus_d = din("caus", [128, 128], F32)
    tb_d = din("tb", [128, S], F32)
    oh_d = din("oh", [32, 4 * 383], F32)
    neg_d = din("negc", [128, 3 * 383], F32)
    oh31_d = din("oh31", [32, 128], F32)
    bitc_d = din("bitc", [128, 32], I32)
    dbc_d = din("dbc", [128, 32], I32)
    out_d = nc.dram_tensor("out", [NB, S, D_MODEL], F32, kind="ExternalOutput").ap()

    QaT_d = dscr("QaT", [NB, 512, S], BF16).ap()
    KaT_d = dscr("KaT", [NB, 512, S], BF16).ap()
    QbT_d = dscr("QbT", [NB, 512, S], BF16).ap()
    KbT_d = dscr("KbT", [NB, 512, S], BF16).ap()
    qiT_d = dscr("qiT", [NB, 256, S], BF16).ap()
    kiT_d = dscr("kiT", [NB, 64, S], BF16).ap()
    Va_d = dscr("Va", [NB, S, 520], BF16).ap()
    Vb_d = dscr("Vb", [NB, S, 520], BF16).ap()
    Ga_d = dscr("Ga", [NB, S, 512], BF16).ap()
    Gb_d = dscr("Gb", [NB, S, 512], BF16).ap()
    MixA_d = dscr("MixA", [NB, S, 512], BF16).ap()
    Bres_d = dscr("Bres", [NB, 3, S, 520], F32).ap()
    Rscr_h = dscr("Rscr", [32 * 128 * 383], F32)
    dQa, dKa, dQb, dKb, dqi, dki = [T(None, n) for n in ("dQa", "dKa", "dQb", "dKb", "dqi", "dki")]
    dVa, dVb, dGa, dGb, dMix, dBres, dR = [T(None, n) for n in ("dVa", "dVb", "dGa", "dGb", "dMix", "dBres", "dR")]

    out_tokens = {}

    with contextlib.ExitStack() as gst:
        P = Prog(nc, gst)

        uniq = [0]

        def mk(st):
            def sb(name, shape, dt):
                uniq[0] += 1
                return T(st.enter_context(nc.sbuf_tensor(f"{name}_u{uniq[0]}", shape, dt)), name)
            return sb
        gsb = mk(gst)
        pb = [T(gst.enter_context(nc.psum_tensor(f"pb{j}", [128, 512], F32)), f"pb{j}") for j in range(7)]
        pT = T(gst.enter_context(nc.psum_tensor("pT", [128, 1024], BF16)), "pT")

        ident = gsb("ident", [128, 128], BF16)
        bones = gsb("bones", [128, 128], BF16)
        caus = gsb("caus", [128, 128], F32)
        bitc = gsb("bitc", [128, 32], I32)
        dbc = gsb("dbc", [128, 32], I32)
        zerot = gsb("zerot", [128, 1], F32)
        gain8 = gsb("gain8", [128, 8], F32)
        qkg = gsb("qkg", [128, 4], F32)
        epst = gsb("epst", [128, 1], F32)
        b31 = gsb("b31", [128, 16], F32)
        wi_all = gsb("wi_all", [128, NB * NT, 4], F32)
        Wo = gsb("Wo", [128, 8, 1024], BF16)
        NBt = [[gsb(f"NB{ty}_{h}", [128, 256], BF16) for h in range(8)] for ty in range(1)]
        EBt = [[gsb(f"EB{p_}_{hp}", [128, 512], BF16) for hp in range(4)] for p_ in range(3)]
        for t_, d_ in ((ident, ident_d), (bones, bones_d), (caus, caus_d), (bitc, bitc_d), (dbc, dbc_d),
                       (gain8, gain8_d), (qkg, qkg_d)):
            P.dma("sp", _dma(t_[:], d_), writes=[t_])
        P.op("dve", lambda e: e.memset(epst[:], EPS), writes=[epst])
        P.op("dve", lambda e: e.memset(zerot[:], 0.0), writes=[zerot])

        with contextlib.ExitStack() as st:
            sb = mk(st)
            relb = sb("relb", [32, 16], F32)
            oh = sb("oh", [32, 4 * 383], F32)
            negc = sb("negc", [128, 3 * 383], F32)
            oh31 = sb("oh31", [32, 128], F32)
            rep = [sb(f"rep{j}", [32, 128], F32) for j in range(2)]
            Rt = [sb(f"Rt{j}", [128, 383], F32) for j in range(2)]
            NBf = [sb(f"NBf{j}", [128, 256], F32) for j in range(2)]
            wst = [sb(f"wst{j}", [128, 8, 512], F32) for j in range(2)]
            W = None
            for t_, d_ in ((relb, relb_d), (oh, oh_d), (negc, neg_d), (oh31, oh31_d)):
                P.dma("sp", _dma(t_[:], d_), writes=[t_])
            P.op("pe", _mm(pb[6][:, 0:16], oh31[:], relb[:], True, True), reads=[oh31, relb], writes=[pb[6]])
            P.op("dve", _cp(b31[:], pb[6][:, 0:16]), reads=[pb[6]], writes=[b31])
            k = 0
            for ty in range(4):
                for h in range(8):
                    col = h if ty == 0 else 8 + h
                    rp = rep[k % 2]
                    rt = Rt[k % 2]
                    nf = NBf[k % 2]
                    pk = pb[k % 2]
                    P.op("dve", _cp(rp[:], relb[:, col:col + 1].to_broadcast([32, 128])), reads=[relb], writes=[rp])
                    P.op("pe", _mm(pk[:, 0:383], rp[:], oh[:, ty * 383:(ty + 1) * 383], True, True),
                         reads=[rp, oh], writes=[pk])
                    if ty == 0:
                        P.op("dve", _ts(rt[:], pk[:, 0:383], b31[:, h:h + 1], 8.0, ALU.subtract, ALU.mult),
                             reads=[pk, b31], writes=[rt])
                    else:
                        P.op("dve", _stt(rt[:], pk[:, 0:383], 8.0, negc[:, (ty - 1) * 383:ty * 383], ALU.mult, ALU.add),
                             reads=[pk, negc], writes=[rt])
                    idx = ty * 8 + h
                    scr_w = bass.AP(Rscr_h, idx * 128 * 383, [[383, 128], [1, 383]])
                    scr_r = bass.AP(Rscr_h, idx * 128 * 383 + 127, [[382, 128], [1, 256]])
                    dRk = T(None, "dRk")
                    P.dma("pool", _dma(scr_w, rt[:]), reads=[rt], writes=[dRk])
                    P.dma("sp", _dma(nf[:], scr_r), reads=[dRk], writes=[nf])
                    if ty == 0:
                        P.op("dve", _cp(NBt[ty][h][:], nf[:]), reads=[nf], writes=[NBt[ty][h]])
                    else:
                        eb = EBt[ty - 1][h // 2]
                        P.op("act", _act(eb[:, (h % 2) * 256:(h % 2 + 1) * 256], nf[:], ACTF.Exp, scale=0.125),
                             reads=[nf], pwrites=[eb])
                    k += 1
            wo_v = wout_d.rearrange("(c p) n -> p c n", p=128)
            for j in range(2):
                ws_ = wst[j % 2]
                P.dma("sp", _dma(ws_[:], wo_v[:, :, j * 512:(j + 1) * 512]), writes=[ws_])
                for c in range(8):
                    P.op("dve", _cp(Wo[:, c, j * 512:(j + 1) * 512], ws_[:, c, :]), reads=[ws_], pwrites=[Wo])
            P.barrier()
            P.flush()

        if True:
            with contextlib.ExitStack() as st2:
                sb2 = mk(st2)
                wst = [sb2(f"wstp{j}", [128, 8, 512], F32) for j in range(2)]
                W = sb2("W", [128, 8, D_IN], BF16)
                wi_v = win_d.rearrange("(c p) n -> p c n", p=128)
                k = 0
                for c0 in range(0, D_IN, 512):
                    nco = min(512, D_IN - c0)
                    ws_ = wst[k % 2]
                    P.dma("sp", _dma(ws_[:, :, :nco], wi_v[:, :, c0:c0 + nco]), writes=[ws_])
                    for c in range(8):
                        P.op("dve", _ts(W[:, c, c0:c0 + nco], ws_[:, c, :nco], gain8[:, c:c + 1], None, ALU.mult),
                             reads=[ws_, gain8], pwrites=[W])
                    k += 1
                xs = [sb2(f"xs{j}", [128, 1024], F32) for j in range(2)]
                xn = [sb2(f"xn{j}", [128, 1024], BF16) for j in range(2)]
                junkx = sb2("junkx", [128, 1024], BF16)
                ss = [sb2(f"ss{j}", [128, 1], F32) for j in range(2)]
                sdx = [sb2(f"sdx{j}", [128, 1], F32) for j in range(2)]
                rsx = [sb2(f"rsx{j}", [128, 1], F32) for j in range(2)]
                xnT = [sb2(f"xnT{j}", [128, 8, 512], BF16) for j in range(2)]
                sqb = [sb2(f"sqb{j}", [128, 512], BF16) for j in range(2)]
                sdb = [sb2(f"sdb{j}", [128, 512], F32) for j in range(2)]
                rsb = [sb2(f"rsb{j}", [128, 512], F32) for j in range(2)]
                fst = [sb2(f"fst{j}", [128, 512], BF16) for j in range(3)]
                vst = [sb2(f"vst{j}", [128, 8, 65], BF16) for j in range(2)]
                gst_ = [sb2(f"gst{j}", [128, 512], BF16) for j in range(2)]
                for v_ in vst:
                    P.op("dve", lambda e, v_=v_: e.memset(v_[:], 1.0), writes=[v_])
                FM = []
                for j in range(4):
                    FM.append((C_QA + 128 * j, 128, QaT_d, dQa, 128 * j, 0))
                for j in range(4):
                    FM.append((C_KA + 128 * j, 128, KaT_d, dKa, 128 * j, 1))
                for j in range(2):
                    FM.append((C_QI + 128 * j, 128, qiT_d, dqi, 128 * j, None))
                FM.append((C_KI, 128, kiT_d, dki, 0, None))
                for j in range(4):
                    FM.append((C_QB + 128 * j, 128, QbT_d, dQb, 128 * j, 2))
                for j in range(4):
                    FM.append((C_KB + 128 * j, 128, KbT_d, dKb, 128 * j, 3))
                TM = [(C_VA, "v", Va_d, dVa), (C_ZA, "g", Ga_d, dGa), (C_VB, "v", Vb_d, dVb), (C_ZB, "g", Gb_d, dGb)]
                kx = 0
                kf = 0
                kt = 0
                kv = 0
                kg_ = 0
                for b in range(NB):
                    for g in range(NG):
                        xg = xnT[(b * NG + g) % 2]
                        for tt in range(4):
                            t = 4 * g + tt
                            xs_, xn_ = xs[kx % 2], xn[kx % 2]
                            ss_, sd_, rs_ = ss[kx % 2], sdx[kx % 2], rsx[kx % 2]
                            kx += 1
                            P.dma("sp", _dma(xs_[:], x_d[b, t * 128:(t + 1) * 128, :]), writes=[xs_])
                            P.op("act", _act(junkx[:], xs_[:], ACTF.Square, accum=ss_[:, 0:1]),
                                 reads=[xs_], writes=[junkx, ss_])
                            P.op("act", _act(sd_[:], ss_[:], ACTF.Sqrt, bias=epst[:, 0:1], scale=1.0 / D_MODEL),
                                 reads=[ss_, epst], writes=[sd_])
                            P.op("dve", lambda e, o=rs_, i=sd_: e.reciprocal(o[:], i[:]), reads=[sd_], writes=[rs_])
                            P.op("dve", _ts(xn_[:], xs_[:], rs_[:, 0:1], None, ALU.mult), reads=[xs_, rs_], writes=[xn_])
                            for c in range(8):
                                P.op("pe", _tr(pT[:, c * 128:(c + 1) * 128], xn_[:, c * 128:(c + 1) * 128], ident[:]),
                                     reads=[xn_, ident], **({"writes": [pT]} if c == 0 else {"pwrites": [pT]}))
                            P.op("act", _acp(xg[:, :, tt * 128:(tt + 1) * 128], pT[:].rearrange("p (c t) -> p c t", c=8)),
                                 reads=[pT], **({"writes": [xg]} if tt == 0 else {"pwrites": [xg]}))
                        for (col0, M, dst, dT_, row0, gc) in FM:
                            pf = pb[kf % 2]
                            for c in range(8):
                                P.op("pe", _mm(pf[0:M, :], W[:, c, col0:col0 + M], xg[:, c, :], c == 0, c == 7),
                                     reads=[W, xg], **({"writes": [pf]} if c == 0 else {"pwrites": [pf]}))
                            fs = fst[kf % 3]
                            if col0 == C_KI:
                                M = 64
                            if gc is not None:
                                sq_, sd2, rs2, pS = sqb[kf % 2], sdb[kf % 2], rsb[kf % 2], pb[2 + kf % 2]
                                P.op("act", _act(sq_[:], pf[:], ACTF.Square), reads=[pf], writes=[sq_])
                                P.op("pe", _mm(pS[:], bones[:], sq_[:], True, True), reads=[bones, sq_], writes=[pS])
                                P.op("act", _act(sd2[:], pS[:], ACTF.Sqrt, bias=epst[:, 0:1]), reads=[pS, epst], writes=[sd2])
                                P.op("dve", lambda e, o=rs2, i=sd2: e.reciprocal(o[:], i[:]), reads=[sd2], writes=[rs2])
                                P.op("dve", _stt(fs[:], pf[:], qkg[:, gc:gc + 1], rs2[:], ALU.mult, ALU.mult),
                                     reads=[pf, qkg, rs2], writes=[fs])
                            else:
                                P.op("act", _acp(fs[0:M, :], pf[0:M, :]), reads=[pf], writes=[fs])
                            P.dma("pool", _dma(dst[b, row0:row0 + M, g * 512:(g + 1) * 512], fs[0:M, :]),
                                  reads=[fs], pwrites=[dT_])
                            kf += 1
                        for tt in range(4):
                            t = 4 * g + tt
                            lt = xg[:, :, tt * 128:(tt + 1) * 128]
                            for (col0, kind, dst, dT_) in TM:
                                pt = pb[4 + kt % 2]
                                kt += 1
                                for c in range(8):
                                    P.op("pe", _mm(pt[:], xg[:, c, tt * 128:(tt + 1) * 128], W[:, c, col0:col0 + 512], c == 0, c == 7),
                                         reads=[W, xg], **({"writes": [pt]} if c == 0 else {"pwrites": [pt]}))
                                if kind == "v":
                                    vs_ = vst[kv % 2]
                                    kv += 1
                                    P.op("dve", _cp(vs_[:, :, 0:64], pt[:].rearrange("p (h d) -> p h d", h=8)),
                                         reads=[pt], pwrites=[vs_])
                                    P.dma("pool", _dma(dst[b, t * 128:(t + 1) * 128, :], vs_[:].rearrange("p h d -> p (h d)")),
                                          reads=[vs_], pwrites=[dT_])
                                else:
                                    gs_ = gst_[kg_ % 2]
                                    kg_ += 1
                                    P.op("act", _act(gs_[:], pt[:], ACTF.Silu), reads=[pt], writes=[gs_])
                                    P.dma("pool", _dma(dst[b, t * 128:(t + 1) * 128, :], gs_[:]), reads=[gs_], pwrites=[dT_])
                            pw = pb[6]
                            for c in range(8):
                                P.op("pe", _mm(pw[:, 0:4], xg[:, c, tt * 128:(tt + 1) * 128], W[:, c, C_WI:C_WI + 4], c == 0, c == 7),
                                     reads=[W, xg], **({"writes": [pw]} if c == 0 else {"pwrites": [pw]}))
                            P.op("dve", _ts(wi_all[:, b * NT + t, :], pw[:, 0:4], WSCALE, None, ALU.mult),
                                 reads=[pw], pwrites=[wi_all])
                P.barrier()
                P.flush()

        for b in range(NB):
            with contextlib.ExitStack() as st:
                sb = mk(st)
                KaT = sb("KaT_s", [128, 4, S], BF16)
                VA = sb("VA_s", [128, NT, 520], BF16)
                kiT2 = sb("kiT2", [128, S], BF16)
                scb = [sb(f"sc{j}", [128, S], F32) for j in range(2)]
                nm = [sb(f"nm{j}", [128, S], BF16) for j in range(4)]
                rbuf = [sb(f"rbuf{j}", [128, 512], F32) for j in range(3)]
                pbuf = [sb(f"pbuf{j}", [128, 512], BF16) for j in range(3)]
                Qi = [sb(f"Qi{j}", [128, 8, 128], BF16) for j in range(4)]
                qii = [sb(f"qii{j}", [128, 4, 128], BF16) for j in range(2)]
                Gi = [sb(f"Gi{j}", [128, 8, 64], BF16) for j in range(4)]
                pvs = [sb(f"pvs{j}", [128, 8, 65], F32) for j in range(2)]
                rden = sb("rden", [128, 8, 1], F32)
                t1 = sb("t1", [128, 8, 64], F32)
                mixed = [sb(f"mixed{j}", [128, 8, 64], BF16) for j in range(2)]
                cntb = [sb(f"cnt{j}", [128, 1], F32) for j in range(2)]
                negmb = [sb(f"negm{j}", [128, 1], I32) for j in range(2)]
                candb = [sb(f"cand{j}", [128, 1], I32) for j in range(2)]
                kbb = [sb(f"kb{j}", [128, 1], I32) for j in range(2)]
                P.dma("sp", _dma(KaT[:], KaT_d[b].rearrange("(c p) s -> p c s", p=128)), reads=[dKa], writes=[KaT])
                P.dma("sp", _dma(VA[:], Va_d[b].rearrange("(t p) f -> p t f", p=128)), reads=[dVa], writes=[VA])
                P.dma("sp", _dma(kiT2[0:64, :], kiT_d[b]), reads=[dki], writes=[kiT2])
                P.dma("sp", _dma(kiT2[64:128, :], kiT_d[b]), reads=[dki], pwrites=[kiT2])
                qa_v = QaT_d[b].rearrange("(c hh p) s -> hh p c s", hh=2, p=64)
                qi_v = qiT_d[b].rearrange("(c hh p) s -> hh p c s", hh=2, p=64)
                for z_ in Qi + qii:
                    P.op("pool", lambda e, z_=z_: e.memset(z_[:], 0.0), writes=[z_])
                cnt_sc = [0]
                cnt_l = [0]

                def scores(i):
                    n = 128 * (i + 1)
                    Q_, q_, G_ = Qi[i % 4], qii[i % 2], Gi[i % 4]
                    sc = scb[i % 2]
                    isl = slice(i * 128, (i + 1) * 128)
                    P.dma("sp", _dma(Q_[0:64, 0:8:2, :], qa_v[0][:, :, isl]), reads=[dQa], pwrites=[Q_])
                    P.dma("sp", _dma(Q_[64:128, 1:8:2, :], qa_v[1][:, :, isl]), reads=[dQa], pwrites=[Q_])
                    P.dma("sp", _dma(q_[0:64, 0:4:2, :], qi_v[0][:, :, isl]), reads=[dqi], pwrites=[q_])
                    P.dma("sp", _dma(q_[64:128, 1:4:2, :], qi_v[1][:, :, isl]), reads=[dqi], pwrites=[q_])
                    P.dma("sp", _dma(G_[:].rearrange("p h d -> p (h d)"), Ga_d[b, i * 128:(i + 1) * 128, :]), reads=[dGa], writes=[G_])
                    P.dma("sp", _dma(sc[:, :n], tb_d[:, :n]), writes=[sc])
                    wcol = b * NT + i
                    for kg in range((n + 511) // 512):
                        ncol = min(512, n - kg * 512)
                        cs = slice(kg * 512, kg * 512 + ncol)
                        for h in range(4):
                            pk = pb[cnt_sc[0] % 2]
                            rb = rbuf[cnt_sc[0] % 3]
                            cnt_sc[0] += 1
                            hh, hc = h % 2, h // 2
                            P.op("pe", _mm(pk[:, :ncol], q_[:, h, :], kiT2[:, cs], True, True),
                                 reads=[q_, kiT2], writes=[pk])
                            P.op("act", _act(rb[:, :ncol], pk[:, :ncol], ACTF.Relu), reads=[pk], writes=[rb])
                            P.op("dve", _stt(sc[:, cs], rb[:, :ncol], wi_all[:, wcol, h:h + 1], sc[:, cs], ALU.mult, ALU.add),
                                 reads=[rb, wi_all, sc], pwrites=[sc])
                        yield
                    dsl = slice(i * 128, (i + 1) * 128)
                    P.op("dve", _tt(sc[:, dsl], sc[:, dsl], caus[:], ALU.add), reads=[sc, caus], pwrites=[sc])

                def bisect(blocks):
                    def count(i, mode, s1, rd):
                        n = 128 * (i + 1)
                        sc, cnt, jk = scb[i % 2], cntb[i % 2], nm[i % 4]
                        if mode == "dve":
                            P.op("dve", _ts(jk[:, :n], sc[:, :n], s1, None, ALU.is_ge, ALU.add, accum=cnt[:, 0:1]),
                                 reads=[sc] + rd, writes=[jk, cnt])
                        else:
                            P.op("act", _act(jk[:, :n], sc[:, :n], ACTF.Sign, bias=s1, scale=-1.0, accum=cnt[:, 0:1]),
                                 reads=[sc] + rd, writes=[jk, cnt])

                    def thrc(i, mode):
                        n = 128 * (i + 1)
                        return (KTH, ALU.is_ge, ALU.is_lt) if mode == "dve" else (-(2.0 * TOPK - n - 1.5), ALU.is_le, ALU.is_gt)
                    for (i, mode) in blocks:
                        if mode == "dve":
                            count(i, mode, 0.0, [])
                        else:
                            count(i, mode, zerot[:, 0:1], [zerot])
                    for (i, mode) in blocks:
                        cnt, negm, cand = cntb[i % 2], negmb[i % 2], candb[i % 2]
                        c, opk, opn = thrc(i, mode)
                        P.op("dve", _ts(negm[:], cnt[:], c, -1.0, opn, ALU.mult), reads=[cnt], writes=[negm])
                        P.op("dve", _stt(cand[:], negm[:], bitc[:, 30:31], bitc[:, 29:30], ALU.bitwise_and, ALU.bitwise_xor),
                             reads=[negm, bitc], writes=[cand])
                    yield
                    for bit in range(29, -1, -1):
                        for (i, mode) in blocks:
                            cand = candb[i % 2]
                            count(i, mode, cand[:, 0:1].bitcast(F32), [cand])
                        for (i, mode) in blocks:
                            cnt, cand, kb = cntb[i % 2], candb[i % 2], kbb[i % 2]
                            c, opk, opn = thrc(i, mode)
                            P.op("dve", _ts(kb[:], cnt[:], c, float(2 ** bit), opk, ALU.mult), reads=[cnt], writes=[kb])
                            P.op("dve", _stt(cand[:], kb[:], dbc[:, bit:bit + 1], cand[:], ALU.bitwise_xor, ALU.bitwise_xor),
                                 reads=[kb, dbc, cand], writes=[cand])
                        yield
                    for (i, mode) in blocks:
                        n = 128 * (i + 1)
                        P.op("dve", _ts(nm[i % 4][:, :n], scb[i % 2][:, :n], candb[i % 2][:, 0:1].bitcast(F32), NEGBIG, ALU.is_lt, ALU.mult),
                             reads=[scb[i % 2], candb[i % 2]], writes=[nm[i % 4]])
                    yield

                def stageA(m):
                    blocks = [i for i in (2 * m, 2 * m + 1) if i < NT]
                    for i in blocks:
                        yield from scores(i)
                    modes = ["dve", "act"]
                    yield from bisect([(i, modes[k]) for k, i in enumerate(blocks)])

                def attend(i):
                    Q_, G_ = Qi[i % 4], Gi[i % 4]
                    nm_ = nm[i % 4]
                    pv_ = pvs[i % 2]
                    ngr = (i + 4) // 4
                    steps = [(h, g) for h in range(8) for g in range(ngr)]
                    base = cnt_l[0]
                    cnt_l[0] += len(steps)

                    def qk(k):
                        h, g = steps[k]
                        pl = pb[2 + (base + k) % 3]
                        hp, hh = h // 2, h % 2
                        chunks = list(range(4 * g, min(4 * g + 4, i + 1)))
                        for kk, c in enumerate(chunks):
                            o = pl[:, kk * 128:(kk + 1) * 128]
                            near = c >= i - 1
                            P.op("pe", _mm(o, KaT[:, hp, c * 128:(c + 1) * 128], Q_[:, h, :], True, False),
                                 reads=[KaT, Q_], **({"writes": [pl]} if kk == 0 else {"pwrites": [pl]}))
                            P.op("pe", _mm(o, nm_[:, c * 128:(c + 1) * 128], ident[:], False, not near),
                                 reads=[nm_, ident], pwrites=[pl])
                            if near:
                                nbs = NBt[0][h][:, 0:128] if c == i else NBt[0][h][:, 128:256]
                                P.op("pe", _mm(o, ident[:], nbs, False, True), reads=[NBt[0][h], ident], pwrites=[pl])

                    def ex(k):
                        h, g = steps[k]
                        pl = pb[2 + (base + k) % 3]
                        pbf = pbuf[(base + k) % 3]
                        ncol = 128 * (min(4 * g + 4, i + 1) - 4 * g)
                        P.op("act", _act(pbf[:, :ncol], pl[:, :ncol], ACTF.Exp, bias=b31[:, h:h + 1], scale=0.125),
                             reads=[pl, b31], writes=[pbf])

                    def pv(k):
                        h, g = steps[k]
                        pbf = pbuf[(base + k) % 3]
                        pp = pb[5 + h % 2]
                        chunks = list(range(4 * g, min(4 * g + 4, i + 1)))
                        for kk, c in enumerate(chunks):
                            P.op("pe", _mm(pp[:, 0:65], pbf[:, kk * 128:(kk + 1) * 128], VA[:, c, h * 65:(h + 1) * 65], c == 0, c == i),
                                 reads=[pbf, VA], **({"writes": [pp]} if c == 0 else {"pwrites": [pp]}))
                        if g == ngr - 1:
                            P.op("act", _acp(pv_[:, h, :], pp[:, 0:65]), reads=[pp],
                                 **({"writes": [pv_]} if h == 0 else {"pwrites": [pv_]}))
                    qk(0)
                    for k in range(len(steps)):
                        if k + 1 < len(steps):
                            qk(k + 1)
                        ex(k)
                        pv(k)
                        yield
                    mx = mixed[i % 2]
                    P.op("dve", lambda e: e.reciprocal(rden[:], pv_[:, :, 64:65]), reads=[pv_], writes=[rden])
                    P.op("dve", _tt(t1[:], pv_[:, :, 0:64], rden[:].to_broadcast([128, 8, 64]), ALU.mult), reads=[pv_, rden], writes=[t1])
                    P.op("dve", _tt(mx[:], t1[:], G_[:], ALU.mult), reads=[t1, G_], writes=[mx])
                    P.dma("pool", _dma(MixA_d[b, i * 128:(i + 1) * 128, :], mx[:].rearrange("p h d -> p (h d)")),
                          reads=[mx], pwrites=[dMix])
                    yield

                def stageB(m):
                    for i in (2 * m, 2 * m + 1):
                        if i < NT:
                            yield from attend(i)

                NP_ = (NT + 1) // 2

                def unitsA(m):
                    u = 0
                    for i in (2 * m, 2 * m + 1):
                        if i < NT:
                            u += (128 * (i + 1) + 511) // 512
                    return u + 32

                def unitsB(m):
                    u = 0
                    for i in (2 * m, 2 * m + 1):
                        if i < NT:
                            u += 8 * ((i + 4) // 4) + 1
                    return u
                for _ in stageA(0):
                    pass
                for m in range(NP_):
                    ga = stageA(m + 1) if m + 1 < NP_ else iter(())
                    gbb = stageB(m)
                    ua = unitsA(m + 1) if m + 1 < NP_ else 0
                    ub = unitsB(m)
                    da = db = 0
                    a_alive, b_alive = ua > 0, True
                    while a_alive or b_alive:
                        if a_alive and (not b_alive or da * ub <= db * ua):
                            try:
                                next(ga)
                                da += 1
                            except StopIteration:
                                a_alive = False
                        else:
                            try:
                                next(gbb)
                                db += 1
                            except StopIteration:
                                b_alive = False
                P.barrier()
                P.flush()

            with contextlib.ExitStack() as st:
                sb = mk(st)
                QbT = sb("QbT_s", [128, 8, S], BF16)
                KbT = sb("KbT_s", [128, 4, S], BF16)
                vt = [sb(f"vt{j}", [128, 520], BF16) for j in range(3)]
                pbuf = [sb(f"pbufb{j}", [128, 512], BF16) for j in range(3)]
                res = [sb(f"res{j}", [128, 520], F32) for j in range(2)]
                P.op("pool", lambda e: e.memset(QbT[:], 0.0), writes=[QbT])
                qb_v = QbT_d[b].rearrange("(c hh p) s -> hh p c s", hh=2, p=64)
                P.dma("sp", _dma(QbT[0:64, 0:8:2, :], qb_v[0]), reads=[dQb], pwrites=[QbT])
                P.dma("sp", _dma(QbT[64:128, 1:8:2, :], qb_v[1]), reads=[dQb], pwrites=[QbT])
                P.dma("sp", _dma(KbT[:], KbT_d[b].rearrange("(c p) s -> p c s", p=128)), reads=[dKb], writes=[KbT])
                tiles = []
                for p_, dil in enumerate(DILS):
                    nbk = S // (dil * 128)
                    for r in range(dil):
                        for c in range(nbk):
                            tiles.append((p_, dil, r, c))
                steps = [(ti, hp) for ti in range(len(tiles)) for hp in range(4)]

                def qsl(dil, r, c):
                    return slice(r + 128 * c * dil, r + 128 * c * dil + 127 * dil + 1, dil)

                def qkB(k):
                    ti, hp = steps[k]
                    p_, dil, r, c = tiles[ti]
                    qs = qsl(dil, r, c)
                    if hp == 0:
                        v_ = vt[ti % 3]
                        P.dma("sp", _dma(v_[:], Vb_d[b, qs, :]), reads=[dVb], writes=[v_])
                    pl = pb[k % 3]
                    firstw = True
                    for hh in range(2):
                        h = 2 * hp + hh
                        bs = hh * 256
                        srcs = [(0, qs)] + ([(128, qsl(dil, r, c - 1))] if c >= 1 else [])
                        for off, ks in srcs:
                            o = pl[:, bs + off:bs + off + 128]
                            P.op("pe", _mm(o, KbT[:, hp, ks], QbT[:, h, qs], True, True),
                                 reads=[KbT, QbT], **({"writes": [pl]} if firstw else {"pwrites": [pl]}))
                            firstw = False

                def exB(k):
                    ti, hp = steps[k]
                    p_, dil, r, c = tiles[ti]
                    pl = pb[k % 3]
                    pbf = pbuf[k % 3]
                    eb = EBt[p_][hp]
                    if c >= 1:
                        P.op("act", _act(pbf[:], pl[:], ACTF.Exp, scale=0.125), reads=[pl], writes=[pbf])
                        P.op("pool", _tt(pbf[:], pbf[:], eb[:], ALU.mult), reads=[pbf, eb], writes=[pbf])
                    else:
                        v3 = lambda t_: t_[:].rearrange("p (a b) -> p a b", a=2)[:, :, 0:128]
                        P.op("act", _act(v3(pbf), v3(pl), ACTF.Exp, scale=0.125), reads=[pl], writes=[pbf])
                        P.op("pool", _tt(v3(pbf), v3(pbf), v3(eb), ALU.mult), reads=[pbf, eb], writes=[pbf])

                def pvB(k):
                    ti, hp = steps[k]
                    p_, dil, r, c = tiles[ti]
                    pbf = pbuf[k % 3]
                    v_ = vt[ti % 3]
                    vp = vt[(ti - 1) % 3]
                    for hh in range(2):
                        h = 2 * hp + hh
                        bs = hh * 256
                        pp = pb[3 + 2 * (ti % 2) + h // 4]
                        col = (h % 4) * 65
                        P.op("pe", _mm(pp[:, col:col + 65], pbf[:, bs:bs + 128], v_[:, h * 65:(h + 1) * 65], True, c == 0),
                             reads=[pbf, v_], **({"writes": [pp]} if h % 4 == 0 else {"pwrites": [pp]}))
                        if c >= 1:
                            P.op("pe", _mm(pp[:, col:col + 65], pbf[:, bs + 128:bs + 256], vp[:, h * 65:(h + 1) * 65], False, True),
                                 reads=[pbf, vp], pwrites=[pp])
                    if hp == 3:
                        rs_ = res[ti % 2]
                        P.op("act", _acp(rs_[:, 0:260], pb[3 + 2 * (ti % 2)][:, 0:260]), reads=[pb[3 + 2 * (ti % 2)]], writes=[rs_])
                        P.op("dve", _cp(rs_[:, 260:520], pb[4 + 2 * (ti % 2)][:, 0:260]), reads=[pb[4 + 2 * (ti % 2)]], pwrites=[rs_])
                        P.dma("pool", _dma(Bres_d[b, p_, qsl(dil, r, c), :], rs_[:]), reads=[rs_], pwrites=[dBres])
                qkB(0)
                for k in range(len(steps)):
                    if k + 1 < len(steps):
                        qkB(k + 1)
                    exB(k)
                    pvB(k)
                P.barrier()
                P.flush()

            with contextlib.ExitStack() as st:
                sb = mk(st)
                xs = [sb(f"xf{j}", [128, 1024], F32) for j in range(2)]
                mix = [sb(f"mix{j}", [128, 1024], BF16) for j in range(2)]
                rr = [[sb(f"rr{j}_{q}", [128, 8, 65], F32) for q in range(3)] for j in range(2)]
                gb = [sb(f"gb{j}", [128, 8, 64], BF16) for j in range(2)]
                ssum = sb("ssum", [128, 8, 65], F32)
                rden = sb("rdenf", [128, 8, 1], F32)
                t1 = sb("t1f", [128, 8, 64], F32)
                mixT = [sb(f"mixT{j}", [128, 1024], BF16) for j in range(2)]
                ot = [sb(f"ot{j}", [128, 1024], F32) for j in range(2)]
                for t in range(NT):
                    j = t % 2
                    rows = slice(t * 128, (t + 1) * 128)
                    P.dma("sp", _dma(xs[j][:], x_d[b, rows, :]), writes=[xs[j]])
                    P.dma("sp", _dma(mix[j][:, 0:512], MixA_d[b, rows, :]), reads=[dMix], writes=[mix[j]])
                    for q in range(3):
                        P.dma("sp", _dma(rr[j][q][:].rearrange("p h d -> p (h d)"), Bres_d[b, q, rows, :]), reads=[dBres], writes=[rr[j][q]])
                    P.dma("sp", _dma(gb[j][:].rearrange("p h d -> p (h d)"), Gb_d[b, rows, :]), reads=[dGb], writes=[gb[j]])
                    P.op("dve", _tt(ssum[:], rr[j][0][:], rr[j][1][:], ALU.add), reads=[rr[j][0], rr[j][1]], writes=[ssum])
                    P.op("dve", _tt(ssum[:], ssum[:], rr[j][2][:], ALU.add), reads=[ssum, rr[j][2]], writes=[ssum])
                    P.op("dve", lambda e: e.reciprocal(rden[:], ssum[:, :, 64:65]), reads=[ssum], writes=[rden])
                    P.op("dve", _tt(t1[:], ssum[:, :, 0:64], rden[:].to_broadcast([128, 8, 64]), ALU.mult), reads=[ssum, rden], writes=[t1])
                    P.op("dve", _tt(mix[j][:, 512:1024].rearrange("p (h d) -> p h d", h=8), t1[:], gb[j][:], ALU.mult),
                         reads=[t1, gb[j]], pwrites=[mix[j]])
                    for c in range(8):
                        P.op("pe", _tr(pT[:, c * 128:(c + 1) * 128], mix[j][:, c * 128:(c + 1) * 128], ident[:]),
                             reads=[mix[j], ident], **({"writes": [pT]} if c == 0 else {"pwrites": [pT]}))
                    P.op("act", _acp(mixT[j][:], pT[:]), reads=[pT], writes=[mixT[j]])
                    for half in range(2):
                        po = pb[2 * j + half]
                        for c in range(8):
                            P.op("pe", _mm(po[:], mixT[j][:, c * 128:(c + 1) * 128], Wo[:, c, half * 512:(half + 1) * 512], c == 0, c == 7),
                                 reads=[mixT[j], Wo], **({"writes": [po]} if c == 0 else {"pwrites": [po]}))
                        P.op("dve", _tt(ot[j][:, half * 512:(half + 1) * 512], po[:], xs[j][:, half * 512:(half + 1) * 512], ALU.add),
                             reads=[po, xs[j]], **({"writes": [ot[j]]} if half == 0 else {"pwrites": [ot[j]]}))
                    tok = P.dma("pool", _dma(out_d[b, rows, :], ot[j][:]), reads=[ot[j]])
                    out_tokens[tok[0]] = max(out_tokens.get(tok[0], 0), tok[1])
                P.barrier()
                P.flush(final_tokens=out_tokens if b == NB - 1 else None)
    return nc


def _rel_bucket(d):
    d = np.maximum(np.asarray(d, np.int64), 0)
    df = np.maximum(d, 1).astype(np.float32)
    large = 16 + (np.log(df / np.float32(16)) / np.float32(math.log(128 / 16)) * np.float32(16)).astype(np.int32)
    large = np.minimum(large, 31)
    return np.where(d < 16, d, large).astype(np.int64)


def host_consts(S):
    bf = ml_dtypes.bfloat16
    c = {}
    c["ident"] = np.eye(128, dtype=np.float32).astype(bf)
    bo = np.zeros((128, 128), np.float32)
    bo[:64, :64] = 1.0 / 64
    bo[64:, 64:] = 1.0 / 64
    c["bones"] = bo.astype(bf)
    q = np.arange(128)[:, None]
    s = np.arange(128)[None, :]
    c["caus"] = np.where(s <= q, 0.0, -4.0).astype(np.float32)
    c["tb"] = np.tile((-(np.arange(S, dtype=np.float64) + 1) * 2.0 ** -100).astype(np.float32)[None], (128, 1))
    oh = np.zeros((32, 4 * 383), np.float32)
    neg = np.zeros((128, 3 * 383), np.float32)
    xs = np.arange(383)
    d = xs - 127
    bk = _rel_bucket(d)
    for x_ in range(383):
        if d[x_] >= 0:
            oh[bk[x_], x_] = 1.0
    for p_, dil in enumerate(DILS):
        valid = (d >= 0) & (d <= 128)
        bkp = _rel_bucket(d * dil)
        for x_ in range(383):
            if valid[x_]:
                oh[bkp[x_], (1 + p_) * 383 + x_] = 1.0
            else:
                neg[:, p_ * 383 + x_] = 8.0 * NEGBIG
    c["oh"] = oh
    c["negc"] = neg
    o31 = np.zeros((32, 128), np.float32)
    o31[31, :] = 1.0
    c["oh31"] = o31
    bc = np.zeros((128, 32), np.int32)
    for b in range(30):
        bc[:, b] = 1 << b
    bc[:, 30] = np.int32(-1073741825)
    c["bitc"] = bc
    db = np.zeros((128, 32), np.int32)
    db[:, 0] = 1
    for b in range(1, 30):
        db[:, b] = (1 << b) ^ (1 << (b - 1))
    c["dbc"] = db
    return c


def host_inputs(S, x_c, norm_gain, w_in, w_out, rel_bias, q_norm_a, k_norm_a, q_norm_b, k_norm_b):
    m = dict(host_consts(S))
    m["x"] = np.ascontiguousarray(x_c, dtype=np.float32)
    m["w_in"] = np.ascontiguousarray(w_in[0], dtype=np.float32)
    m["w_out"] = np.ascontiguousarray(w_out[0], dtype=np.float32)
    m["gain8"] = np.ascontiguousarray(np.asarray(norm_gain[0], np.float32).reshape(8, 128).T)
    m["relb"] = np.ascontiguousarray(rel_bias, dtype=np.float32)
    m["qkg"] = np.ascontiguousarray(np.stack(
        [np.tile(np.asarray(g[0], np.float32), 2) for g in (q_norm_a, k_norm_a, q_norm_b, k_norm_b)], axis=1))
    return m


_NC_CACHE = {}


def kernel(x, norm_gain, w_in, w_out, rel_bias, q_norm_a, k_norm_a, q_norm_b, k_norm_b):
    x = np.asarray(x)
    B, S, D = x.shape
    n = 8
    NB = B // n
    key = (S, NB)
    if key not in _NC_CACHE:
        _NC_CACHE[key] = build(S, NB)
    nc = _NC_CACHE[key]
    args = [np.asarray(a) for a in (norm_gain, w_in, w_out, rel_bias, q_norm_a, k_norm_a, q_norm_b, k_norm_b)]
    in_maps = [host_inputs(S, x[k * NB:(k + 1) * NB], *args) for k in range(n)]
    res = run_bass_kernel_spmd(nc, in_maps, core_ids=list(range(n)))
    return np.concatenate([r["out"] for r in res.results], axis=0).astype(np.float32)
```

```python
import math
import contextlib
import numpy as np
import ml_dtypes
import concourse.bass as bass
import concourse.mybir as mybir
from concourse.bass_utils import run_bass_kernel_spmd

F32 = mybir.dt.float32
BF16 = mybir.dt.bfloat16
I32 = mybir.dt.int32
ALU = mybir.AluOpType
ACTF = mybir.ActivationFunctionType

NDMA_SEM = 12
D_MODEL = 1024
D_IN = 4420
EPS = 1e-6
C_QA, C_KA, C_VA, C_ZA, C_QI, C_KI, C_WI, C_QB, C_KB, C_VB, C_ZB = (
    0, 512, 1024, 1536, 2048, 2304, 2368, 2372, 2884, 3396, 3908)
DILS = (1, 4, 16)
NEGBIG = -30000.0
WSCALE = 2.0 ** -12


class T:
    __slots__ = ("t", "writers", "readers", "name", "full")

    def __init__(self, t, name=""):
        self.t = t
        self.writers = []
        self.readers = []
        self.name = name
        self.full = None

    def __getitem__(self, k):
        return self.t[k]


class Prog:
    ENGS = ("pe", "act", "dve", "pool", "sp")

    def __init__(self, nc, st):
        self.nc = nc
        self.ops = {e: [] for e in self.ENGS}
        self.count = {e: 0 for e in self.ENGS}
        self.seen = {e: {} for e in self.ENGS}
        self.dma_n = {"sp": 0, "pool": 0}
        self.dma_last = {}
        self.pending = {e: [] for e in self.ENGS}
        self.n_instr = 0
        self.sems = {}
        for e in ("pe", "act", "dve", "pool"):
            self.sems[e] = st.enter_context(nc.semaphore("s_" + e))
        for q in ("sp", "pool"):
            for j in range(NDMA_SEM):
                self.sems[("dma", q, j)] = st.enter_context(nc.semaphore(f"d_{q}{j}"))

    def _deps(self, eng, reads, writes, pwrites):
        deps = {}

        def add(tok):
            k, v = tok
            if deps.get(k, 0) < v:
                deps[k] = v
        for t in reads:
            for w in t.writers:
                add(w)
        for t in writes:
            for w in t.writers:
                add(w)
            for r in t.readers:
                add(r)
        for t in pwrites:
            for r in t.readers:
                add(r)
            if t.full is not None:
                add(t.full)
        if self.pending[eng]:
            for tok in self.pending[eng]:
                add(tok)
            self.pending[eng] = []
        return deps

    def _commit(self, tok, reads, writes, pwrites):
        for t in reads:
            t.readers.append(tok)
        for t in writes:
            t.writers = [tok]
            t.readers = []
            t.full = tok
        for t in pwrites:
            t.writers.append(tok)

    def _waits(self, eng, deps):
        ws = []
        seen = self.seen[eng]
        for k, v in deps.items():
            if k == "pe" and eng == "pe":
                continue
            if seen.get(k, 0) < v:
                seen[k] = v
                ws.append((k, v))
        return ws

    def op(self, eng, fn, reads=(), writes=(), pwrites=()):
        deps = self._deps(eng, reads, writes, pwrites)
        ws = self._waits(eng, deps)
        self.count[eng] += 1
        tok = (eng, self.count[eng])
        self.ops[eng].append((1, fn, ws, tok))
        self._commit(tok, reads, writes, pwrites)
        self.n_instr += 1
        return tok

    def dma(self, q, fn, reads=(), writes=(), pwrites=()):
        deps = self._deps(q, reads, writes, pwrites)
        n = self.dma_n[q]
        self.dma_n[q] = n + 1
        key = ("dma", q, n % NDMA_SEM)
        val = 16 * (n // NDMA_SEM + 1)
        if n >= NDMA_SEM and deps.get(key, 0) < val - 16:
            deps[key] = val - 16
        ws = self._waits(q, deps)
        tok = (key, val)
        self.dma_last[key] = val
        self.ops[q].append((16, fn, ws, tok))
        self._commit(tok, reads, writes, pwrites)
        self.n_instr += 1
        return tok

    def barrier(self):
        toks = [(e, self.count[e]) for e in ("pe", "act", "dve", "pool") if self.count[e]]
        toks += list(self.dma_last.items())
        for e in self.ENGS:
            self.pending[e] = list(toks)

    def flush(self, final_tokens=None):
        nc = self.nc
        sems = self.sems
        ops = self.ops
        self.ops = {e: [] for e in self.ENGS}

        def run(name, eng):
            for inc, fn, ws, tok in ops[name]:
                for k, v in ws:
                    eng.wait_ge(sems[k], v)
                fn(eng).then_inc(sems[tok[0]], inc)
            if name == "sp" and final_tokens:
                for k, v in final_tokens.items():
                    eng.wait_ge(sems[k], v)

        with nc.Block() as block:
            @block.tensor
            def _(e):
                run("pe", e)

            @block.scalar
            def _(e):
                run("act", e)

            @block.vector
            def _(e):
                run("dve", e)

            @block.gpsimd
            def _(e):
                run("pool", e)

            @block.sync
            def _(e):
                run("sp", e)


def _mm(o, l, r, start, stop):
    return lambda e: e.matmul(o, lhsT=l, rhs=r, start=start, stop=stop)


def _act(o, i, func, bias=None, scale=1.0, accum=None):
    kw = {}
    if bias is not None:
        kw["bias"] = bias
    if accum is not None:
        kw["accum_out"] = accum
    return lambda e: e.activation(out=o, in_=i, func=func, scale=scale, **kw)


def _ts(o, i, s1, s2, op0, op1=None, accum=None):
    kw = {}
    if op1 is not None:
        kw["op1"] = op1
    if accum is not None:
        kw["accum_out"] = accum
    return lambda e: e.tensor_scalar(out=o, in0=i, scalar1=s1, scalar2=s2, op0=op0, **kw)


def _tt(o, a, b, op):
    return lambda e: e.tensor_tensor(out=o, in0=a, in1=b, op=op)


def _stt(o, a, s, b, op0, op1):
    return lambda e: e.scalar_tensor_tensor(out=o, in0=a, scalar=s, in1=b, op0=op0, op1=op1)


def _cp(o, i):
    return lambda e: e.tensor_copy(out=o, in_=i)


def _acp(o, i):
    return lambda e: e.activation(out=o, in_=i, func=ACTF.Copy)


def _dma(o, i):
    return lambda e: e.dma_start(out=o, in_=i)


def _tr(o, i, ident):
    return lambda e: e.transpose(o, i, ident)


def build(S, NB):
    TOPK = min(256, S // 4)
    KTH = float(TOPK) - 0.5
    NT = S // 128
    NG = S // 512
    nc = bass.Bass("TRN2", target_bir_lowering=False)

    def din(name, shape, dt):
        return nc.dram_tensor(name, shape, dt, kind="ExternalInput").ap()

    def dscr(name, shape, dt):
        return nc.dram_tensor(name, shape, dt, kind="Internal")

    x_d = din("x", [NB, S, D_MODEL], F32)
    win_d = din("w_in", [D_MODEL, D_IN], F32)
    wout_d = din("w_out", [D_MODEL, D_MODEL], F32)
    gain8_d = din("gain8", [128, 8], F32)
    relb_d = din("relb", [32, 16], F32)
    qkg_d = din("qkg", [128, 4], F32)
    ident_d = din("ident", [128, 128], BF16)
    bones_d = din("bones", [128, 128], BF16)
    caus_d = din("caus", [128, 128], F32)
    tb_d = din("tb", [128, S], F32)
    oh_d = din("oh", [32, 4 * 383], F32)
    neg_d = din("negc", [128, 3 * 383], F32)
    oh31_d = din("oh31", [32, 128], F32)
    bitc_d = din("bitc", [128, 32], I32)
    dbc_d = din("dbc", [128, 32], I32)
    out_d = nc.dram_tensor("out", [NB, S, D_MODEL], F32, kind="ExternalOutput").ap()

    QaT_d = dscr("QaT", [NB, 512, S], BF16).ap()
    KaT_d = dscr("KaT", [NB, 512, S], BF16).ap()
    QbT_d = dscr("QbT", [NB, 512, S], BF16).ap()
    KbT_d = dscr("KbT", [NB, 512, S], BF16).ap()
    qiT_d = dscr("qiT", [NB, 256, S], BF16).ap()
    kiT_d = dscr("kiT", [NB, 64, S], BF16).ap()
    Va_d = dscr("Va", [NB, S, 520], BF16).ap()
    Vb_d = dscr("Vb", [NB, S, 520], BF16).ap()
    Ga_d = dscr("Ga", [NB, S, 512], BF16).ap()
    Gb_d = dscr("Gb", [NB, S, 512], BF16).ap()
    MixA_d = dscr("MixA", [NB, S, 512], BF16).ap()
    Bres_d = dscr("Bres", [NB, 3, S, 520], F32).ap()
    Rscr_h = dscr("Rscr", [32 * 128 * 383], F32)
    dQa, dKa, dQb, dKb, dqi, dki = [T(None, n) for n in ("dQa", "dKa", "dQb", "dKb", "dqi", "dki")]
    dVa, dVb, dGa, dGb, dMix, dBres, dR = [T(None, n) for n in ("dVa", "dVb", "dGa", "dGb", "dMix", "dBres", "dR")]

    out_tokens = {}

    with contextlib.ExitStack() as gst:
        P = Prog(nc, gst)

        uniq = [0]

        def mk(st):
            def sb(name, shape, dt):
                uniq[0] += 1
                return T(st.enter_context(nc.sbuf_tensor(f"{name}_u{uniq[0]}", shape, dt)), name)
            return sb
        gsb = mk(gst)
        pb = [T(gst.enter_context(nc.psum_tensor(f"pb{j}", [128, 512], F32)), f"pb{j}") for j in range(7)]
        pT = T(gst.enter_context(nc.psum_tensor("pT", [128, 1024], BF16)), "pT")

        ident = gsb("ident", [128, 128], BF16)
        bones = gsb("bones", [128, 128], BF16)
        caus = gsb("caus", [128, 128], F32)
        bitc = gsb("bitc", [128, 32], I32)
        dbc = gsb("dbc", [128, 32], I32)
        zerot = gsb("zerot", [128, 1], F32)
        gain8 = gsb("gain8", [128, 8], F32)
        qkg = gsb("qkg", [128, 4], F32)
        epst = gsb("epst", [128, 1], F32)
        b31 = gsb("b31", [128, 16], F32)
        wi_all = gsb("wi_all", [128, NB * NT, 4], F32)
        Wo = gsb("Wo", [128, 8, 1024], BF16)
        NBt = [[gsb(f"NB{ty}_{h}", [128, 256], BF16) for h in range(8)] for ty in range(1)]
        EBt = [[gsb(f"EB{p_}_{hp}", [128, 512], BF16) for hp in range(4)] for p_ in range(3)]
        for t_, d_ in ((ident, ident_d), (bones, bones_d), (caus, caus_d), (bitc, bitc_d), (dbc, dbc_d),
                       (gain8, gain8_d), (qkg, qkg_d)):
            P.dma("sp", _dma(t_[:], d_), writes=[t_])
        P.op("dve", lambda e: e.memset(epst[:], EPS), writes=[epst])
        P.op("dve", lambda e: e.memset(zerot[:], 0.0), writes=[zerot])

        with contextlib.ExitStack() as st:
            sb = mk(st)
            sb2 = sb
            relb = sb("relb", [32, 16], F32)
            oh = sb("oh", [32, 4 * 383], F32)
            negc = sb("negc", [128, 3 * 383], F32)
            oh31 = sb("oh31", [32, 128], F32)
            rep = [sb(f"rep{j}", [32, 128], F32) for j in range(2)]
            Rt = [sb(f"Rt{j}", [128, 383], F32) for j in range(2)]
            NBf = [sb(f"NBf{j}", [128, 256], F32) for j in range(2)]
            wst = [sb(f"wst{j}", [128, 8, 512], F32) for j in range(2)]
            W = sb2("W", [128, 8, D_IN], BF16)
            wi_v = win_d.rearrange("(c p) n -> p c n", p=128)
            k = 0
            for c0 in range(0, D_IN, 512):
                nco = min(512, D_IN - c0)
                ws_ = wst[k % 2]
                P.dma("sp", _dma(ws_[:, :, :nco], wi_v[:, :, c0:c0 + nco]), writes=[ws_])
                for c in range(8):
                    P.op("dve" if c % 2 == 0 else "pool", _ts(W[:, c, c0:c0 + nco], ws_[:, c, :nco], gain8[:, c:c + 1], 1.0, ALU.mult, ALU.mult),
                         reads=[ws_, gain8], pwrites=[W])
                k += 1
            wo_v = wout_d.rearrange("(c p) n -> p c n", p=128)
            for j in range(2):
                ws_ = wst[k % 2]
                k += 1
                P.dma("sp", _dma(ws_[:], wo_v[:, :, j * 512:(j + 1) * 512]), writes=[ws_])
                for c in range(8):
                    P.op("dve" if c % 2 == 0 else "pool", _cp(Wo[:, c, j * 512:(j + 1) * 512], ws_[:, c, :]), reads=[ws_], pwrites=[Wo])
            for t_, d_ in ((relb, relb_d), (oh, oh_d), (negc, neg_d), (oh31, oh31_d)):
                P.dma("sp", _dma(t_[:], d_), writes=[t_])

            nbk = [0]

            def nb_b31():
                P.op("pe", _mm(pb[6][:, 0:16], oh31[:], relb[:], True, True), reads=[oh31, relb], writes=[pb[6]])
                P.op("dve", _cp(b31[:], pb[6][:, 0:16]), reads=[pb[6]], writes=[b31])

            def nb_tile(ty, h):
                k = nbk[0]
                nbk[0] += 1
                col = h if ty == 0 else 8 + h
                rp = rep[k % 2]
                rt = Rt[k % 2]
                nf = NBf[k % 2]
                pk = pb[6]
                P.op("dve", _cp(rp[:], relb[:, col:col + 1].to_broadcast([32, 128])), reads=[relb], writes=[rp])
                P.op("pe", _mm(pk[:, 0:383], rp[:], oh[:, ty * 383:(ty + 1) * 383], True, True),
                     reads=[rp, oh], writes=[pk])
                if ty == 0:
                    P.op("dve", _ts(rt[:], pk[:, 0:383], b31[:, h:h + 1], 8.0, ALU.subtract, ALU.mult),
                         reads=[pk, b31], writes=[rt])
                else:
                    P.op("dve", _stt(rt[:], pk[:, 0:383], 8.0, negc[:, (ty - 1) * 383:ty * 383], ALU.mult, ALU.add),
                         reads=[pk, negc], writes=[rt])
                idx = ty * 8 + h
                scr_w = bass.AP(Rscr_h, idx * 128 * 383, [[383, 128], [1, 383]])
                scr_r = bass.AP(Rscr_h, idx * 128 * 383 + 127, [[382, 128], [1, 256]])
                dRk = T(None, "dRk")
                P.dma("pool", _dma(scr_w, rt[:]), reads=[rt], writes=[dRk])
                P.dma("sp", _dma(nf[:], scr_r), reads=[dRk], writes=[nf])
                if ty == 0:
                    P.op("dve", _cp(NBt[ty][h][:], nf[:]), reads=[nf], writes=[NBt[ty][h]])
                else:
                    eb = EBt[ty - 1][h // 2]
                    P.op("act", _act(eb[:, (h % 2) * 256:(h % 2 + 1) * 256], nf[:], ACTF.Exp, scale=0.125),
                         reads=[nf], pwrites=[eb])
            nb_list = [(ty, h) for ty in range(4) for h in range(8)]

            xs = [sb2(f"xs{j}", [128, 1024], F32) for j in range(2)]
            xn = [sb2(f"xn{j}", [128, 1024], BF16) for j in range(2)]
            junkx = sb2("junkx", [128, 1024], BF16)
            ss = [sb2(f"ss{j}", [128, 1], F32) for j in range(2)]
            sdx = [sb2(f"sdx{j}", [128, 1], F32) for j in range(2)]
            rsx = [sb2(f"rsx{j}", [128, 1], F32) for j in range(2)]
            xnT = [sb2(f"xnT{j}", [128, 8, 512], BF16) for j in range(2)]
            sqb = [sb2(f"sqb{j}", [128, 512], BF16) for j in range(2)]
            sdb = [sb2(f"sdb{j}", [128, 512], F32) for j in range(2)]
            rsb = [sb2(f"rsb{j}", [128, 512], F32) for j in range(2)]
            fst = [sb2(f"fst{j}", [128, 512], BF16) for j in range(3)]
            vst = [sb2(f"vst{j}", [128, 8, 65], BF16) for j in range(2)]
            gst_ = [sb2(f"gst{j}", [128, 512], BF16) for j in range(2)]
            for v_ in vst:
                P.op("dve", lambda e, v_=v_: e.memset(v_[:], 1.0), writes=[v_])
            FM = []
            for j in range(4):
                FM.append((C_QA + 128 * j, 128, QaT_d, dQa, 128 * j, 0))
            for j in range(4):
                FM.append((C_KA + 128 * j, 128, KaT_d, dKa, 128 * j, 1))
            for j in range(2):
                FM.append((C_QI + 128 * j, 128, qiT_d, dqi, 128 * j, None))
            FM.append((C_KI, 128, kiT_d, dki, 0, None))
            for j in range(4):
                FM.append((C_QB + 128 * j, 128, QbT_d, dQb, 128 * j, 2))
            for j in range(4):
                FM.append((C_KB + 128 * j, 128, KbT_d, dKb, 128 * j, 3))
            TM = [(C_VA, "v", Va_d, dVa), (C_ZA, "g", Ga_d, dGa), (C_VB, "v", Vb_d, dVb), (C_ZB, "g", Gb_d, dGb)]
            cn = {"kx": 0, "kf": 0, "kt": 0, "kv": 0, "kg": 0}
            groups = [(b, g) for b in range(NB) for g in range(NG)]

            def prep_norm(q, tt):
                b, g = groups[q]
                t = 4 * g + tt
                kx = q * 4 + tt
                xs_, xn_ = xs[kx % 2], xn[kx % 2]
                ss_, sd_, rs_ = ss[kx % 2], sdx[kx % 2], rsx[kx % 2]
                P.dma("sp", _dma(xs_[:], x_d[b, t * 128:(t + 1) * 128, :]), writes=[xs_])
                P.op("act", _act(junkx[:], xs_[:], ACTF.Square, accum=ss_[:, 0:1]),
                     reads=[xs_], writes=[junkx, ss_])
                P.op("act", _act(sd_[:], ss_[:], ACTF.Ln, bias=epst[:, 0:1], scale=1.0 / D_MODEL),
                     reads=[ss_, epst], writes=[sd_])
                P.op("act", _act(rs_[:], sd_[:], ACTF.Exp, scale=-0.5), reads=[sd_], writes=[rs_])
                P.op("dve", _ts(xn_[:], xs_[:], rs_[:, 0:1], None, ALU.mult), reads=[xs_, rs_], writes=[xn_])

            def prep_tr(q, tt):
                kx = q * 4 + tt
                xn_ = xn[kx % 2]
                xg = xnT[q % 2]
                for c in range(8):
                    P.op("pe", _tr(pT[:, c * 128:(c + 1) * 128], xn_[:, c * 128:(c + 1) * 128], ident[:]),
                         reads=[xn_, ident], **({"writes": [pT]} if c == 0 else {"pwrites": [pT]}))
                P.op("act", _acp(xg[:, :, tt * 128:(tt + 1) * 128], pT[:].rearrange("p (c t) -> p c t", c=8)),
                     reads=[pT], **({"writes": [xg]} if tt == 0 else {"pwrites": [xg]}))

            def fm_item(q, idx):
                b, g = groups[q]
                xg = xnT[q % 2]
                (col0, M, dst, dT_, row0, gc) = FM[idx]
                kf = cn["kf"]
                cn["kf"] += 1
                pf = pb[kf % 2]
                for c in range(8):
                    P.op("pe", _mm(pf[0:M, :], W[:, c, col0:col0 + M], xg[:, c, :], c == 0, c == 7),
                         reads=[W, xg], **({"writes": [pf]} if c == 0 else {"pwrites": [pf]}))
                fs = fst[kf % 3]
                if col0 == C_KI:
                    M = 64
                if gc is not None:
                    sq_, sd2, rs2, pS = sqb[kf % 2], sdb[kf % 2], rsb[kf % 2], pb[2 + kf % 2]
                    P.op("act", _act(sq_[:], pf[:], ACTF.Square), reads=[pf], writes=[sq_])
                    P.op("pe", _mm(pS[:], bones[:], sq_[:], True, True), reads=[bones, sq_], writes=[pS])
                    P.op("act", _act(sd2[:], pS[:], ACTF.Ln, bias=epst[:, 0:1]), reads=[pS, epst], writes=[sd2])
                    P.op("act", _act(rs2[:], sd2[:], ACTF.Exp, scale=-0.5), reads=[sd2], writes=[rs2])
                    P.op("dve", _stt(fs[:], pf[:], qkg[:, gc:gc + 1], rs2[:], ALU.mult, ALU.mult),
                         reads=[pf, qkg, rs2], writes=[fs])
                else:
                    P.op("dve", _cp(fs[0:M, :], pf[0:M, :]), reads=[pf], writes=[fs])
                P.dma("pool", _dma(dst[b, row0:row0 + M, g * 512:(g + 1) * 512], fs[0:M, :]),
                      reads=[fs], pwrites=[dT_])

            def tm_item(q, tt, j):
                b, g = groups[q]
                xg = xnT[q % 2]
                t = 4 * g + tt
                (col0, kind, dst, dT_) = TM[j]
                pt = pb[4 + cn["kt"] % 2]
                cn["kt"] += 1
                for c in range(8):
                    P.op("pe", _mm(pt[:], xg[:, c, tt * 128:(tt + 1) * 128], W[:, c, col0:col0 + 512], c == 0, c == 7),
                         reads=[W, xg], **({"writes": [pt]} if c == 0 else {"pwrites": [pt]}))
                if kind == "v":
                    vs_ = vst[cn["kv"] % 2]
                    cn["kv"] += 1
                    P.op("dve", _cp(vs_[:, :, 0:64], pt[:].rearrange("p (h d) -> p h d", h=8)),
                         reads=[pt], pwrites=[vs_])
                    P.dma("pool", _dma(dst[b, t * 128:(t + 1) * 128, :], vs_[:].rearrange("p h d -> p (h d)")),
                          reads=[vs_], pwrites=[dT_])
                else:
                    gs_ = gst_[cn["kg"] % 2]
                    cn["kg"] += 1
                    P.op("act", _act(gs_[:], pt[:], ACTF.Silu), reads=[pt], writes=[gs_])
                    P.dma("pool", _dma(dst[b, t * 128:(t + 1) * 128, :], gs_[:]), reads=[gs_], pwrites=[dT_])
                if j == 3:
                    pw = pb[6]
                    for c in range(8):
                        P.op("pe", _mm(pw[:, 0:4], xg[:, c, tt * 128:(tt + 1) * 128], W[:, c, C_WI:C_WI + 4], c == 0, c == 7),
                             reads=[W, xg], **({"writes": [pw]} if c == 0 else {"pwrites": [pw]}))
                    P.op("dve", _ts(wi_all[:, b * NT + t, :], pw[:, 0:4], WSCALE, None, ALU.mult),
                         reads=[pw], pwrites=[wi_all])

            for tt in range(4):
                prep_norm(0, tt)
                prep_tr(0, tt)
            nb_b31()
            nbi = 0
            per_g = (len(nb_list) + len(groups) - 1) // len(groups)
            for q in range(len(groups)):
                items = []
                fi, ti = 0, 0
                while fi < len(FM) or ti < 16:
                    if fi < len(FM):
                        items.append(("f", fi))
                        fi += 1
                    if ti < 16:
                        items.append(("t", ti))
                        ti += 1
                for n_, (kind, v) in enumerate(items):
                    if q + 1 < len(groups):
                        if n_ >= 2 and (n_ - 2) % 8 == 0 and (n_ - 2) // 8 < 4:
                            prep_norm(q + 1, (n_ - 2) // 8)
                        if n_ >= 6 and (n_ - 6) % 8 == 0 and (n_ - 6) // 8 < 4:
                            prep_tr(q + 1, (n_ - 6) // 8)
                    if kind == "f":
                        fm_item(q, v)
                    else:
                        tm_item(q, v // 4, v % 4)
                    if n_ in (10, 20, 30)[:per_g + 1] and nbi < len(nb_list):
                        nb_tile(*nb_list[nbi])
                        nbi += 1
            while nbi < len(nb_list):
                nb_tile(*nb_list[nbi])
                nbi += 1
            P.barrier()
            P.flush()

        for b in range(NB):
            with contextlib.ExitStack() as st:
                sb = mk(st)
                KaT = sb("KaT_s", [128, 4, S], BF16)
                VA = sb("VA_s", [128, NT, 520], BF16)
                kiT2 = sb("kiT2", [128, S], BF16)
                scb = [sb(f"sc{j}", [128, S], F32) for j in range(2)]
                nm = [sb(f"nm{j}", [128, S], BF16) for j in range(4)]
                rbuf = [sb(f"rbuf{j}", [128, 512], F32) for j in range(3)]
                pbuf = [sb(f"pbuf{j}", [128, 512], BF16) for j in range(3)]
                Qi = [sb(f"Qi{j}", [128, 8, 128], BF16) for j in range(4)]
                qii = [sb(f"qii{j}", [128, 4, 128], BF16) for j in range(2)]
                Gi = [sb(f"Gi{j}", [128, 8, 64], BF16) for j in range(4)]
                pvs = [sb(f"pvs{j}", [128, 8, 65], F32) for j in range(2)]
                rden = sb("rden", [128, 8, 1], F32)
                t1 = sb("t1", [128, 8, 64], F32)
                mixed = [sb(f"mixed{j}", [128, 8, 64], BF16) for j in range(2)]
                cntb = [sb(f"cnt{j}", [128, 1], F32) for j in range(2)]
                negmb = [sb(f"negm{j}", [128, 1], I32) for j in range(2)]
                candb = [sb(f"cand{j}", [128, 1], I32) for j in range(2)]
                kbb = [sb(f"kb{j}", [128, 1], I32) for j in range(2)]
                P.dma("sp", _dma(KaT[:], KaT_d[b].rearrange("(c p) s -> p c s", p=128)), reads=[dKa], writes=[KaT])
                P.dma("sp", _dma(VA[:], Va_d[b].rearrange("(t p) f -> p t f", p=128)), reads=[dVa], writes=[VA])
                P.dma("sp", _dma(kiT2[0:64, :], kiT_d[b]), reads=[dki], writes=[kiT2])
                P.dma("sp", _dma(kiT2[64:128, :], kiT_d[b]), reads=[dki], pwrites=[kiT2])
                qa_v = QaT_d[b].rearrange("(c hh p) s -> hh p c s", hh=2, p=64)
                qi_v = qiT_d[b].rearrange("(c hh p) s -> hh p c s", hh=2, p=64)
                for z_ in Qi + qii:
                    P.op("pool", lambda e, z_=z_: e.memset(z_[:], 0.0), writes=[z_])
                cnt_sc = [0]
                cnt_l = [0]

                def scores(i):
                    n = 128 * (i + 1)
                    Q_, q_, G_ = Qi[i % 4], qii[i % 2], Gi[i % 4]
                    sc = scb[i % 2]
                    isl = slice(i * 128, (i + 1) * 128)
                    P.dma("sp", _dma(Q_[0:64, 0:8:2, :], qa_v[0][:, :, isl]), reads=[dQa], pwrites=[Q_])
                    P.dma("sp", _dma(Q_[64:128, 1:8:2, :], qa_v[1][:, :, isl]), reads=[dQa], pwrites=[Q_])
                    P.dma("sp", _dma(q_[0:64, 0:4:2, :], qi_v[0][:, :, isl]), reads=[dqi], pwrites=[q_])
                    P.dma("sp", _dma(q_[64:128, 1:4:2, :], qi_v[1][:, :, isl]), reads=[dqi], pwrites=[q_])
                    P.dma("sp", _dma(G_[:].rearrange("p h d -> p (h d)"), Ga_d[b, i * 128:(i + 1) * 128, :]), reads=[dGa], writes=[G_])
                    P.dma("sp", _dma(sc[:, :n], tb_d[:, :n]), writes=[sc])
                    wcol = b * NT + i
                    for kg in range((n + 511) // 512):
                        ncol = min(512, n - kg * 512)
                        cs = slice(kg * 512, kg * 512 + ncol)
                        for h in range(4):
                            pk = pb[cnt_sc[0] % 2]
                            rb = rbuf[cnt_sc[0] % 3]
                            cnt_sc[0] += 1
                            hh, hc = h % 2, h // 2
                            P.op("pe", _mm(pk[:, :ncol], q_[:, h, :], kiT2[:, cs], True, True),
                                 reads=[q_, kiT2], writes=[pk])
                            P.op("act", _act(rb[:, :ncol], pk[:, :ncol], ACTF.Relu), reads=[pk], writes=[rb])
                            P.op("dve", _stt(sc[:, cs], rb[:, :ncol], wi_all[:, wcol, h:h + 1], sc[:, cs], ALU.mult, ALU.add),
                                 reads=[rb, wi_all, sc], pwrites=[sc])
                        yield
                    dsl = slice(i * 128, (i + 1) * 128)
                    P.op("dve", _tt(sc[:, dsl], sc[:, dsl], caus[:], ALU.add), reads=[sc, caus], pwrites=[sc])

                def bisect(blocks):
                    def count(i, mode, s1, rd):
                        n = 128 * (i + 1)
                        sc, cnt, jk = scb[i % 2], cntb[i % 2], nm[i % 4]
                        if mode == "dve":
                            P.op("dve", _ts(jk[:, :n], sc[:, :n], s1, None, ALU.is_ge, ALU.add, accum=cnt[:, 0:1]),
                                 reads=[sc] + rd, writes=[jk, cnt])
                        else:
                            P.op("act", _act(jk[:, :n], sc[:, :n], ACTF.Sign, bias=s1, scale=-1.0, accum=cnt[:, 0:1]),
                                 reads=[sc] + rd, writes=[jk, cnt])

                    def thrc(i, mode):
                        n = 128 * (i + 1)
                        return (KTH, ALU.is_ge, ALU.is_lt) if mode == "dve" else (-(2.0 * TOPK - n - 1.5), ALU.is_le, ALU.is_gt)
                    for (i, mode) in blocks:
                        if mode == "dve":
                            count(i, mode, 0.0, [])
                        else:
                            count(i, mode, zerot[:, 0:1], [zerot])
                    for (i, mode) in blocks:
                        cnt, negm, cand = cntb[i % 2], negmb[i % 2], candb[i % 2]
                        c, opk, opn = thrc(i, mode)
                        P.op("dve", _ts(negm[:], cnt[:], c, -1.0, opn, ALU.mult), reads=[cnt], writes=[negm])
                        P.op("dve", _stt(cand[:], negm[:], bitc[:, 30:31], bitc[:, 29:30], ALU.bitwise_and, ALU.bitwise_xor),
                             reads=[negm, bitc], writes=[cand])
                    yield
                    for bit in range(29, -1, -1):
                        for (i, mode) in blocks:
                            cand = candb[i % 2]
                            count(i, mode, cand[:, 0:1].bitcast(F32), [cand])
                        for (i, mode) in blocks:
                            cnt, cand, kb = cntb[i % 2], candb[i % 2], kbb[i % 2]
                            c, opk, opn = thrc(i, mode)
                            P.op("dve", _ts(kb[:], cnt[:], c, float(2 ** bit), opk, ALU.mult), reads=[cnt], writes=[kb])
                            P.op("dve", _stt(cand[:], kb[:], dbc[:, bit:bit + 1], cand[:], ALU.bitwise_xor, ALU.bitwise_xor),
                                 reads=[kb, dbc, cand], writes=[cand])
                        yield
                    for (i, mode) in blocks:
                        n = 128 * (i + 1)
                        P.op("dve", _ts(nm[i % 4][:, :n], scb[i % 2][:, :n], candb[i % 2][:, 0:1].bitcast(F32), NEGBIG, ALU.is_lt, ALU.mult),
                             reads=[scb[i % 2], candb[i % 2]], writes=[nm[i % 4]])
                    yield

                def stageA(m):
                    blocks = [i for i in (2 * m, 2 * m + 1) if i < NT]
                    for i in blocks:
                        yield from scores(i)
                    modes = ["dve", "act"]
                    yield from bisect([(i, modes[k]) for k, i in enumerate(blocks)])

                def attend(i):
                    Q_, G_ = Qi[i % 4], Gi[i % 4]
                    nm_ = nm[i % 4]
                    pv_ = pvs[i % 2]
                    ngr = (i + 4) // 4
                    steps = [(h, g) for h in range(8) for g in range(ngr)]
                    base = cnt_l[0]
                    cnt_l[0] += len(steps)

                    def qk(k):
                        h, g = steps[k]
                        pl = pb[2 + (base + k) % 3]
                        hp, hh = h // 2, h % 2
                        chunks = list(range(4 * g, min(4 * g + 4, i + 1)))
                        for kk, c in enumerate(chunks):
                            o = pl[:, kk * 128:(kk + 1) * 128]
                            near = c >= i - 1
                            P.op("pe", _mm(o, KaT[:, hp, c * 128:(c + 1) * 128], Q_[:, h, :], True, False),
                                 reads=[KaT, Q_], **({"writes": [pl]} if kk == 0 else {"pwrites": [pl]}))
                            P.op("pe", _mm(o, nm_[:, c * 128:(c + 1) * 128], ident[:], False, not near),
                                 reads=[nm_, ident], pwrites=[pl])
                            if near:
                                nbs = NBt[0][h][:, 0:128] if c == i else NBt[0][h][:, 128:256]
                                P.op("pe", _mm(o, ident[:], nbs, False, True), reads=[NBt[0][h], ident], pwrites=[pl])

                    def ex(k):
                        h, g = steps[k]
                        pl = pb[2 + (base + k) % 3]
                        pbf = pbuf[(base + k) % 3]
                        ncol = 128 * (min(4 * g + 4, i + 1) - 4 * g)
                        P.op("act", _act(pbf[:, :ncol], pl[:, :ncol], ACTF.Exp, bias=b31[:, h:h + 1], scale=0.125),
                             reads=[pl, b31], writes=[pbf])

                    def pv(k):
                        h, g = steps[k]
                        pbf = pbuf[(base + k) % 3]
                        pp = pb[5 + h % 2]
                        chunks = list(range(4 * g, min(4 * g + 4, i + 1)))
                        for kk, c in enumerate(chunks):
                            P.op("pe", _mm(pp[:, 0:65], pbf[:, kk * 128:(kk + 1) * 128], VA[:, c, h * 65:(h + 1) * 65], c == 0, c == i),
                                 reads=[pbf, VA], **({"writes": [pp]} if c == 0 else {"pwrites": [pp]}))
                        if g == ngr - 1:
                            P.op("act", _acp(pv_[:, h, :], pp[:, 0:65]), reads=[pp],
                                 **({"writes": [pv_]} if h == 0 else {"pwrites": [pv_]}))
                    qk(0)
                    for k in range(len(steps)):
                        if k + 1 < len(steps):
                            qk(k + 1)
                        ex(k)
                        pv(k)
                        yield
                    mx = mixed[i % 2]
                    P.op("dve", lambda e: e.reciprocal(rden[:], pv_[:, :, 64:65]), reads=[pv_], writes=[rden])
                    P.op("dve", _tt(t1[:], pv_[:, :, 0:64], rden[:].to_broadcast([128, 8, 64]), ALU.mult), reads=[pv_, rden], writes=[t1])
                    P.op("dve", _tt(mx[:], t1[:], G_[:], ALU.mult), reads=[t1, G_], writes=[mx])
                    P.dma("pool", _dma(MixA_d[b, i * 128:(i + 1) * 128, :], mx[:].rearrange("p h d -> p (h d)")),
                          reads=[mx], pwrites=[dMix])
                    yield

                def stageB(m):
                    for i in (2 * m, 2 * m + 1):
                        if i < NT:
                            yield from attend(i)

                NP_ = (NT + 1) // 2

                def unitsA(m):
                    u = 0
                    for i in (2 * m, 2 * m + 1):
                        if i < NT:
                            u += (128 * (i + 1) + 511) // 512
                    return u + 32

                def unitsB(m):
                    u = 0
                    for i in (2 * m, 2 * m + 1):
                        if i < NT:
                            u += 8 * ((i + 4) // 4) + 1
                    return u
                for _ in stageA(0):
                    pass
                for m in range(NP_):
                    ga = stageA(m + 1) if m + 1 < NP_ else iter(())
                    gbb = stageB(m)
                    ua = unitsA(m + 1) if m + 1 < NP_ else 0
                    ub = unitsB(m)
                    da = db = 0
                    a_alive, b_alive = ua > 0, True
                    while a_alive or b_alive:
                        if a_alive and (not b_alive or da * ub <= db * ua):
                            try:
                                next(ga)
                                da += 1
                            except StopIteration:
                                a_alive = False
                        else:
                            try:
                                next(gbb)
                                db += 1
                            except StopIteration:
                                b_alive = False
                P.barrier()
                P.flush()

            with contextlib.ExitStack() as st:
                sb = mk(st)
                QbT = sb("QbT_s", [128, 8, S], BF16)
                KbT = sb("KbT_s", [128, 4, S], BF16)
                vt = [sb(f"vt{j}", [128, 520], BF16) for j in range(3)]
                pbuf = [sb(f"pbufb{j}", [128, 512], BF16) for j in range(3)]
                res = [sb(f"res{j}", [128, 520], F32) for j in range(2)]
                P.op("pool", lambda e: e.memset(QbT[:], 0.0), writes=[QbT])
                qb_v = QbT_d[b].rearrange("(c hh p) s -> hh p c s", hh=2, p=64)
                P.dma("sp", _dma(QbT[0:64, 0:8:2, :], qb_v[0]), reads=[dQb], pwrites=[QbT])
                P.dma("sp", _dma(QbT[64:128, 1:8:2, :], qb_v[1]), reads=[dQb], pwrites=[QbT])
                P.dma("sp", _dma(KbT[:], KbT_d[b].rearrange("(c p) s -> p c s", p=128)), reads=[dKb], writes=[KbT])
                tiles = []
                for p_, dil in enumerate(DILS):
                    nbk = S // (dil * 128)
                    for r in range(dil):
                        for c in range(nbk):
                            tiles.append((p_, dil, r, c))
                steps = [(ti, hp) for ti in range(len(tiles)) for hp in range(4)]

                def qsl(dil, r, c):
                    return slice(r + 128 * c * dil, r + 128 * c * dil + 127 * dil + 1, dil)

                def qkB(k):
                    ti, hp = steps[k]
                    p_, dil, r, c = tiles[ti]
                    qs = qsl(dil, r, c)
                    if hp == 0:
                        v_ = vt[ti % 3]
                        P.dma("sp", _dma(v_[:], Vb_d[b, qs, :]), reads=[dVb], writes=[v_])
                    pl = pb[k % 3]
                    firstw = True
                    for hh in range(2):
                        h = 2 * hp + hh
                        bs = hh * 256
                        srcs = [(0, qs)] + ([(128, qsl(dil, r, c - 1))] if c >= 1 else [])
                        for off, ks in srcs:
                            o = pl[:, bs + off:bs + off + 128]
                            P.op("pe", _mm(o, KbT[:, hp, ks], QbT[:, h, qs], True, True),
                                 reads=[KbT, QbT], **({"writes": [pl]} if firstw else {"pwrites": [pl]}))
                            firstw = False

                def exB(k):
                    ti, hp = steps[k]
                    p_, dil, r, c = tiles[ti]
                    pl = pb[k % 3]
                    pbf = pbuf[k % 3]
                    eb = EBt[p_][hp]
                    if c >= 1:
                        P.op("act", _act(pbf[:], pl[:], ACTF.Exp, scale=0.125), reads=[pl], writes=[pbf])
                        P.op("dve", _tt(pbf[:], pbf[:], eb[:], ALU.mult), reads=[pbf, eb], writes=[pbf])
                    else:
                        v3 = lambda t_: t_[:].rearrange("p (a b) -> p a b", a=2)[:, :, 0:128]
                        P.op("act", _act(v3(pbf), v3(pl), ACTF.Exp, scale=0.125), reads=[pl], writes=[pbf])
                        P.op("dve", _tt(v3(pbf), v3(pbf), v3(eb), ALU.mult), reads=[pbf, eb], writes=[pbf])

                def pvB(k):
                    ti, hp = steps[k]
                    p_, dil, r, c = tiles[ti]
                    pbf = pbuf[k % 3]
                    v_ = vt[ti % 3]
                    vp = vt[(ti - 1) % 3]
                    for hh in range(2):
                        h = 2 * hp + hh
                        bs = hh * 256
                        pp = pb[3 + 2 * (ti % 2) + h // 4]
                        col = (h % 4) * 65
                        P.op("pe", _mm(pp[:, col:col + 65], pbf[:, bs:bs + 128], v_[:, h * 65:(h + 1) * 65], True, c == 0),
                             reads=[pbf, v_], **({"writes": [pp]} if h % 4 == 0 else {"pwrites": [pp]}))
                        if c >= 1:
                            P.op("pe", _mm(pp[:, col:col + 65], pbf[:, bs + 128:bs + 256], vp[:, h * 65:(h + 1) * 65], False, True),
                                 reads=[pbf, vp], pwrites=[pp])
                    if hp == 3:
                        rs_ = res[ti % 2]
                        P.op("act", _acp(rs_[:, 0:260], pb[3 + 2 * (ti % 2)][:, 0:260]), reads=[pb[3 + 2 * (ti % 2)]], writes=[rs_])
                        P.op("dve", _cp(rs_[:, 260:520], pb[4 + 2 * (ti % 2)][:, 0:260]), reads=[pb[4 + 2 * (ti % 2)]], pwrites=[rs_])
                        P.dma("pool", _dma(Bres_d[b, p_, qsl(dil, r, c), :], rs_[:]), reads=[rs_], pwrites=[dBres])
                nst = len(steps)
                qkB(0)
                if nst > 1:
                    qkB(1)
                exB(0)
                for k in range(nst):
                    if k + 2 < nst:
                        qkB(k + 2)
                    if k + 1 < nst:
                        exB(k + 1)
                    pvB(k)
                P.barrier()
                P.flush()

            with contextlib.ExitStack() as st:
                sb = mk(st)
                xs = [sb(f"xf{j}", [128, 1024], F32) for j in range(2)]
                mix = [sb(f"mix{j}", [128, 1024], BF16) for j in range(2)]
                rr = [[sb(f"rr{j}_{q}", [128, 8, 65], F32) for q in range(3)] for j in range(2)]
                gb = [sb(f"gb{j}", [128, 8, 64], BF16) for j in range(2)]
                ssum = [sb(f"ssum{j}", [128, 8, 65], F32) for j in range(2)]
                rden = [sb(f"rdenf{j}", [128, 8, 1], F32) for j in range(2)]
                t1 = [sb(f"t1f{j}", [128, 8, 64], F32) for j in range(2)]
                mixT = [sb(f"mixT{j}", [128, 1024], BF16) for j in range(2)]
                ot = [sb(f"ot{j}", [128, 1024], F32) for j in range(2)]

                def F1(t):
                    j = t % 2
                    rows = slice(t * 128, (t + 1) * 128)
                    P.dma("sp", _dma(xs[j][:], x_d[b, rows, :]), writes=[xs[j]])
                    P.dma("sp", _dma(mix[j][:, 0:512], MixA_d[b, rows, :]), reads=[dMix], writes=[mix[j]])
                    for q in range(3):
                        P.dma("sp", _dma(rr[j][q][:].rearrange("p h d -> p (h d)"), Bres_d[b, q, rows, :]), reads=[dBres], writes=[rr[j][q]])
                    P.dma("sp", _dma(gb[j][:].rearrange("p h d -> p (h d)"), Gb_d[b, rows, :]), reads=[dGb], writes=[gb[j]])
                    sm, rd, tt1 = ssum[j], rden[j], t1[j]
                    P.op("dve", _tt(sm[:], rr[j][0][:], rr[j][1][:], ALU.add), reads=[rr[j][0], rr[j][1]], writes=[sm])
                    P.op("dve", _tt(sm[:], sm[:], rr[j][2][:], ALU.add), reads=[sm, rr[j][2]], writes=[sm])
                    P.op("dve", lambda e: e.reciprocal(rd[:], sm[:, :, 64:65]), reads=[sm], writes=[rd])
                    P.op("dve", _tt(tt1[:], sm[:, :, 0:64], rd[:].to_broadcast([128, 8, 64]), ALU.mult), reads=[sm, rd], writes=[tt1])
                    P.op("dve", _tt(mix[j][:, 512:1024].rearrange("p (h d) -> p h d", h=8), tt1[:], gb[j][:], ALU.mult),
                         reads=[tt1, gb[j]], pwrites=[mix[j]])

                def F2(t):
                    j = t % 2
                    rows = slice(t * 128, (t + 1) * 128)
                    for c in range(8):
                        P.op("pe", _tr(pT[:, c * 128:(c + 1) * 128], mix[j][:, c * 128:(c + 1) * 128], ident[:]),
                             reads=[mix[j], ident], **({"writes": [pT]} if c == 0 else {"pwrites": [pT]}))
                    P.op("act", _acp(mixT[j][:], pT[:]), reads=[pT], writes=[mixT[j]])
                    for half in range(2):
                        po = pb[2 * j + half]
                        for c in range(8):
                            P.op("pe", _mm(po[:], mixT[j][:, c * 128:(c + 1) * 128], Wo[:, c, half * 512:(half + 1) * 512], c == 0, c == 7),
                                 reads=[mixT[j], Wo], **({"writes": [po]} if c == 0 else {"pwrites": [po]}))
                        P.op("dve", _tt(ot[j][:, half * 512:(half + 1) * 512], po[:], xs[j][:, half * 512:(half + 1) * 512], ALU.add),
                             reads=[po, xs[j]], **({"writes": [ot[j]]} if half == 0 else {"pwrites": [ot[j]]}))
                    tok = P.dma("pool", _dma(out_d[b, rows, :], ot[j][:]), reads=[ot[j]])
                    out_tokens[tok[0]] = max(out_tokens.get(tok[0], 0), tok[1])
                F1(0)
                for t in range(NT):
                    if t + 1 < NT:
                        F1(t + 1)
                    F2(t)
                P.barrier()
                P.flush(final_tokens=out_tokens if b == NB - 1 else None)
    return nc


def _rel_bucket(d):
    d = np.maximum(np.asarray(d, np.int64), 0)
    df = np.maximum(d, 1).astype(np.float32)
    large = 16 + (np.log(df / np.float32(16)) / np.float32(math.log(128 / 16)) * np.float32(16)).astype(np.int32)
    large = np.minimum(large, 31)
    return np.where(d < 16, d, large).astype(np.int64)


def host_consts(S):
    bf = ml_dtypes.bfloat16
    c = {}
    c["ident"] = np.eye(128, dtype=np.float32).astype(bf)
    bo = np.zeros((128, 128), np.float32)
    bo[:64, :64] = 1.0 / 64
    bo[64:, 64:] = 1.0 / 64
    c["bones"] = bo.astype(bf)
    q = np.arange(128)[:, None]
    s = np.arange(128)[None, :]
    c["caus"] = np.where(s <= q, 0.0, -4.0).astype(np.float32)
    c["tb"] = np.tile((-(np.arange(S, dtype=np.float64) + 1) * 2.0 ** -100).astype(np.float32)[None], (128, 1))
    oh = np.zeros((32, 4 * 383), np.float32)
    neg = np.zeros((128, 3 * 383), np.float32)
    xs = np.arange(383)
    d = xs - 127
    bk = _rel_bucket(d)
    for x_ in range(383):
        if d[x_] >= 0:
            oh[bk[x_], x_] = 1.0
    for p_, dil in enumerate(DILS):
        valid = (d >= 0) & (d <= 128)
        bkp = _rel_bucket(d * dil)
        for x_ in range(383):
            if valid[x_]:
                oh[bkp[x_], (1 + p_) * 383 + x_] = 1.0
            else:
                neg[:, p_ * 383 + x_] = 8.0 * NEGBIG
    c["oh"] = oh
    c["negc"] = neg
    o31 = np.zeros((32, 128), np.float32)
    o31[31, :] = 1.0
    c["oh31"] = o31
    bc = np.zeros((128, 32), np.int32)
    for b in range(30):
        bc[:, b] = 1 << b
    bc[:, 30] = np.int32(-1073741825)
    c["bitc"] = bc
    db = np.zeros((128, 32), np.int32)
    db[:, 0] = 1
    for b in range(1, 30):
        db[:, b] = (1 << b) ^ (1 << (b - 1))
    c["dbc"] = db
    return c


def host_inputs(S, x_c, norm_gain, w_in, w_out, rel_bias, q_norm_a, k_norm_a, q_norm_b, k_norm_b):
    m = dict(host_consts(S))
    m["x"] = np.ascontiguousarray(x_c, dtype=np.float32)
    m["w_in"] = np.ascontiguousarray(w_in[0], dtype=np.float32)
    m["w_out"] = np.ascontiguousarray(w_out[0], dtype=np.float32)
    m["gain8"] = np.ascontiguousarray(np.asarray(norm_gain[0], np.float32).reshape(8, 128).T)
    m["relb"] = np.ascontiguousarray(rel_bias, dtype=np.float32)
    m["qkg"] = np.ascontiguousarray(np.stack(
        [np.tile(np.asarray(g[0], np.float32), 2) for g in (q_norm_a, k_norm_a, q_norm_b, k_norm_b)], axis=1))
    return m


_NC_CACHE = {}


def kernel(x, norm_gain, w_in, w_out, rel_bias, q_norm_a, k_norm_a, q_norm_b, k_norm_b):
    x = np.asarray(x)
    B, S, D = x.shape
    n = 8
    NB = B // n
    key = (S, NB)
    if key not in _NC_CACHE:
        _NC_CACHE[key] = build(S, NB)
    nc = _NC_CACHE[key]
    args = [np.asarray(a) for a in (norm_gain, w_in, w_out, rel_bias, q_norm_a, k_norm_a, q_norm_b, k_norm_b)]
    in_maps = [host_inputs(S, x[k * NB:(k + 1) * NB], *args) for k in range(n)]
    res = run_bass_kernel_spmd(nc, in_maps, core_ids=list(range(n)))
    return np.concatenate([r["out"] for r in res.results], axis=0).astype(np.float32)
```

```python
import math
import contextlib
import numpy as np
import ml_dtypes
import concourse.bass as bass
import concourse.mybir as mybir
from concourse.bass_utils import run_bass_kernel_spmd

F32 = mybir.dt.float32
BF16 = mybir.dt.bfloat16
I32 = mybir.dt.int32
ALU = mybir.AluOpType
ACTF = mybir.ActivationFunctionType

NDMA_SEM = 12
D_MODEL = 1024
D_IN = 4420
EPS = 1e-6
C_QA, C_KA, C_VA, C_ZA, C_QI, C_KI, C_WI, C_QB, C_KB, C_VB, C_ZB = (
    0, 512, 1024, 1536, 2048, 2304, 2368, 2372, 2884, 3396, 3908)
DILS = (1, 4, 16)
NEGBIG = -30000.0
WSCALE = 2.0 ** -12


class T:
    __slots__ = ("t", "writers", "readers", "name", "full")

    def __init__(self, t, name=""):
        self.t = t
        self.writers = []
        self.readers = []
        self.name = name
        self.full = None

    def __getitem__(self, k):
        return self.t[k]


class Prog:
    ENGS = ("pe", "act", "dve", "pool", "sp")

    def __init__(self, nc, st):
        self.nc = nc
        self.ops = {e: [] for e in self.ENGS}
        self.count = {e: 0 for e in self.ENGS}
        self.seen = {e: {} for e in self.ENGS}
        self.dma_n = {"sp": 0, "pool": 0}
        self.dma_last = {}
        self.pending = {e: [] for e in self.ENGS}
        self.n_instr = 0
        self.sems = {}
        for e in ("pe", "act", "dve", "pool"):
            self.sems[e] = st.enter_context(nc.semaphore("s_" + e))
        for q in ("sp", "pool"):
            for j in range(NDMA_SEM):
                self.sems[("dma", q, j)] = st.enter_context(nc.semaphore(f"d_{q}{j}"))

    def _deps(self, eng, reads, writes, pwrites):
        deps = {}

        def add(tok):
            k, v = tok
            if deps.get(k, 0) < v:
                deps[k] = v
        for t in reads:
            for w in t.writers:
                add(w)
        for t in writes:
            for w in t.writers:
                add(w)
            for r in t.readers:
                add(r)
        for t in pwrites:
            for r in t.readers:
                add(r)
            if t.full is not None:
                add(t.full)
        if self.pending[eng]:
            for tok in self.pending[eng]:
                add(tok)
            self.pending[eng] = []
        return deps

    def _commit(self, tok, reads, writes, pwrites):
        for t in reads:
            t.readers.append(tok)
        for t in writes:
            t.writers = [tok]
            t.readers = []
            t.full = tok
        for t in pwrites:
            t.writers.append(tok)

    def _waits(self, eng, deps):
        ws = []
        seen = self.seen[eng]
        for k, v in deps.items():
            if k == "pe" and eng == "pe":
                continue
            if seen.get(k, 0) < v:
                seen[k] = v
                ws.append((k, v))
        return ws

    def op(self, eng, fn, reads=(), writes=(), pwrites=()):
        deps = self._deps(eng, reads, writes, pwrites)
        ws = self._waits(eng, deps)
        self.count[eng] += 1
        tok = (eng, self.count[eng])
        self.ops[eng].append((1, fn, ws, tok))
        self._commit(tok, reads, writes, pwrites)
        self.n_instr += 1
        return tok

    def dma(self, q, fn, reads=(), writes=(), pwrites=()):
        deps = self._deps(q, reads, writes, pwrites)
        n = self.dma_n[q]
        self.dma_n[q] = n + 1
        key = ("dma", q, n % NDMA_SEM)
        val = 16 * (n // NDMA_SEM + 1)
        if n >= NDMA_SEM and deps.get(key, 0) < val - 16:
            deps[key] = val - 16
        ws = self._waits(q, deps)
        tok = (key, val)
        self.dma_last[key] = val
        self.ops[q].append((16, fn, ws, tok))
        self._commit(tok, reads, writes, pwrites)
        self.n_instr += 1
        return tok

    def barrier(self):
        toks = [(e, self.count[e]) for e in ("pe", "act", "dve", "pool") if self.count[e]]
        toks += list(self.dma_last.items())
        for e in self.ENGS:
            self.pending[e] = list(toks)

    def flush(self, final_tokens=None):
        nc = self.nc
        sems = self.sems
        ops = self.ops
        self.ops = {e: [] for e in self.ENGS}

        def run(name, eng):
            for inc, fn, ws, tok in ops[name]:
                for k, v in ws:
                    eng.wait_ge(sems[k], v)
                fn(eng).then_inc(sems[tok[0]], inc)
            if name == "sp" and final_tokens:
                for k, v in final_tokens.items():
                    eng.wait_ge(sems[k], v)

        with nc.Block() as block:
            @block.tensor
            def _(e):
                run("pe", e)

            @block.scalar
            def _(e):
                run("act", e)

            @block.vector
            def _(e):
                run("dve", e)

            @block.gpsimd
            def _(e):
                run("pool", e)

            @block.sync
            def _(e):
                run("sp", e)


def _mm(o, l, r, start, stop):
    return lambda e: e.matmul(o, lhsT=l, rhs=r, start=start, stop=stop)


def _act(o, i, func, bias=None, scale=1.0, accum=None):
    kw = {}
    if bias is not None:
        kw["bias"] = bias
    if accum is not None:
        kw["accum_out"] = accum
    return lambda e: e.activation(out=o, in_=i, func=func, scale=scale, **kw)


def _ts(o, i, s1, s2, op0, op1=None, accum=None):
    kw = {}
    if op1 is not None:
        kw["op1"] = op1
    if accum is not None:
        kw["accum_out"] = accum
    return lambda e: e.tensor_scalar(out=o, in0=i, scalar1=s1, scalar2=s2, op0=op0, **kw)


def _tt(o, a, b, op):
    return lambda e: e.tensor_tensor(out=o, in0=a, in1=b, op=op)


def _stt(o, a, s, b, op0, op1):
    return lambda e: e.scalar_tensor_tensor(out=o, in0=a, scalar=s, in1=b, op0=op0, op1=op1)


def _cp(o, i):
    return lambda e: e.tensor_copy(out=o, in_=i)


def _acp(o, i):
    return lambda e: e.activation(out=o, in_=i, func=ACTF.Copy)


def _dma(o, i):
    return lambda e: e.dma_start(out=o, in_=i)


def _tr(o, i, ident):
    return lambda e: e.transpose(o, i, ident)


def build(S, NB):
    TOPK = min(256, S // 4)
    KTH = float(TOPK) - 0.5
    NT = S // 128
    NG = S // 512
    nc = bass.Bass("TRN2", target_bir_lowering=False)

    def din(name, shape, dt):
        return nc.dram_tensor(name, shape, dt, kind="ExternalInput").ap()

    def dscr(name, shape, dt):
        return nc.dram_tensor(name, shape, dt, kind="Internal")

    x_d = din("x", [NB, S, D_MODEL], F32)
    win_d = din("w_in", [D_MODEL, D_IN], F32)
    wout_d = din("w_out", [D_MODEL, D_MODEL], F32)
    gain8_d = din("gain8", [128, 8], F32)
    relb_d = din("relb", [32, 16], F32)
    qkg_d = din("qkg", [128, 4], F32)
    ident_d = din("ident", [128, 128], BF16)
    bones_d = din("bones", [128, 128], BF16)
    caus_d = din("caus", [128, 128], F32)
    tb_d = din("tb", [128, S], F32)
    oh_d = din("oh", [32, 4 * 383], F32)
    neg_d = din("negc", [128, 3 * 383], F32)
    oh31_d = din("oh31", [32, 128], F32)
    bitc_d = din("bitc", [128, 32], I32)
    dbc_d = din("dbc", [128, 32], I32)
    out_d = nc.dram_tensor("out", [NB, S, D_MODEL], F32, kind="ExternalOutput").ap()

    QaT_d = dscr("QaT", [NB, 512, S], BF16).ap()
    KaT_d = dscr("KaT", [NB, 512, S], BF16).ap()
    QbT_d = dscr("QbT", [NB, 512, S], BF16).ap()
    KbT_d = dscr("KbT", [NB, 512, S], BF16).ap()
    qiT_d = dscr("qiT", [NB, 256, S], BF16).ap()
    kiT_d = dscr("kiT", [NB, 64, S], BF16).ap()
    Va_d = dscr("Va", [NB, S, 520], BF16).ap()
    Vb_d = dscr("Vb", [NB, S, 520], BF16).ap()
    Ga_d = dscr("Ga", [NB, S, 512], BF16).ap()
    Gb_d = dscr("Gb", [NB, S, 512], BF16).ap()
    MixA_d = dscr("MixA", [NB, S, 512], BF16).ap()
    Bres_d = dscr("Bres", [NB, 3, S, 520], F32).ap()
    Rscr_h = dscr("Rscr", [32 * 128 * 383], F32)
    dQa, dKa, dQb, dKb, dqi, dki = [T(None, n) for n in ("dQa", "dKa", "dQb", "dKb", "dqi", "dki")]
    dVa, dVb, dGa, dGb, dMix, dBres, dR = [T(None, n) for n in ("dVa", "dVb", "dGa", "dGb", "dMix", "dBres", "dR")]

    out_tokens = {}

    with contextlib.ExitStack() as gst:
        P = Prog(nc, gst)

        uniq = [0]

        def mk(st):
            def sb(name, shape, dt):
                uniq[0] += 1
                return T(st.enter_context(nc.sbuf_tensor(f"{name}_u{uniq[0]}", shape, dt)), name)
            return sb
        gsb = mk(gst)
        pb = [T(gst.enter_context(nc.psum_tensor(f"pb{j}", [128, 512], F32)), f"pb{j}") for j in range(7)]
        pT = T(gst.enter_context(nc.psum_tensor("pT", [128, 1024], BF16)), "pT")

        ident = gsb("ident", [128, 128], BF16)
        bones = gsb("bones", [128, 128], BF16)
        caus = gsb("caus", [128, 128], F32)
        bitc = gsb("bitc", [128, 32], I32)
        dbc = gsb("dbc", [128, 32], I32)
        zerot = gsb("zerot", [128, 1], F32)
        gain8 = gsb("gain8", [128, 8], F32)
        qkg = gsb("qkg", [128, 4], F32)
        epst = gsb("epst", [128, 1], F32)
        b31 = gsb("b31", [128, 16], F32)
        wi_all = gsb("wi_all", [128, NB * NT, 4], F32)
        Wo = gsb("Wo", [128, 8, 1024], BF16)
        NBt = [[gsb(f"NB{ty}_{h}", [128, 256], BF16) for h in range(8)] for ty in range(1)]
        EBt = [[gsb(f"EB{p_}_{hp}", [128, 512], BF16) for hp in range(4)] for p_ in range(3)]
        for t_, d_ in ((ident, ident_d), (bones, bones_d), (caus, caus_d), (bitc, bitc_d), (dbc, dbc_d),
                       (gain8, gain8_d), (qkg, qkg_d)):
            P.dma("sp", _dma(t_[:], d_), writes=[t_])
        P.op("dve", lambda e: e.memset(epst[:], EPS), writes=[epst])
        P.op("dve", lambda e: e.memset(zerot[:], 0.0), writes=[zerot])

        with contextlib.ExitStack() as st:
            sb = mk(st)
            sb2 = sb
            relb = sb("relb", [32, 16], F32)
            oh = sb("oh", [32, 4 * 383], F32)
            negc = sb("negc", [128, 3 * 383], F32)
            oh31 = sb("oh31", [32, 128], F32)
            rep = [sb(f"rep{j}", [32, 128], F32) for j in range(2)]
            Rt = [sb(f"Rt{j}", [128, 383], F32) for j in range(2)]
            NBf = [sb(f"NBf{j}", [128, 256], F32) for j in range(2)]
            wst = [sb(f"wst{j}", [128, 8, 512], F32) for j in range(2)]
            W = sb2("W", [128, 8, D_IN], BF16)
            wi_v = win_d.rearrange("(c p) n -> p c n", p=128)
            k = 0
            for c0 in range(0, D_IN, 512):
                nco = min(512, D_IN - c0)
                ws_ = wst[k % 2]
                P.dma("sp", _dma(ws_[:, :, :nco], wi_v[:, :, c0:c0 + nco]), writes=[ws_])
                for c in range(8):
                    P.op("dve" if c % 2 == 0 else "pool", _ts(W[:, c, c0:c0 + nco], ws_[:, c, :nco], gain8[:, c:c + 1], 1.0, ALU.mult, ALU.mult),
                         reads=[ws_, gain8], pwrites=[W])
                k += 1
            wo_v = wout_d.rearrange("(c p) n -> p c n", p=128)
            for j in range(2):
                ws_ = wst[k % 2]
                k += 1
                P.dma("sp", _dma(ws_[:], wo_v[:, :, j * 512:(j + 1) * 512]), writes=[ws_])
                for c in range(8):
                    P.op("dve" if c % 2 == 0 else "pool", _cp(Wo[:, c, j * 512:(j + 1) * 512], ws_[:, c, :]), reads=[ws_], pwrites=[Wo])
            for t_, d_ in ((relb, relb_d), (oh, oh_d), (negc, neg_d), (oh31, oh31_d)):
                P.dma("sp", _dma(t_[:], d_), writes=[t_])

            nbk = [0]

            def nb_b31():
                P.op("pe", _mm(pb[6][:, 0:16], oh31[:], relb[:], True, True), reads=[oh31, relb], writes=[pb[6]])
                P.op("dve", _cp(b31[:], pb[6][:, 0:16]), reads=[pb[6]], writes=[b31])

            def nb_tile(ty, h):
                k = nbk[0]
                nbk[0] += 1
                col = h if ty == 0 else 8 + h
                rp = rep[k % 2]
                rt = Rt[k % 2]
                nf = NBf[k % 2]
                pk = pb[6]
                P.op("dve", _cp(rp[:], relb[:, col:col + 1].to_broadcast([32, 128])), reads=[relb], writes=[rp])
                P.op("pe", _mm(pk[:, 0:383], rp[:], oh[:, ty * 383:(ty + 1) * 383], True, True),
                     reads=[rp, oh], writes=[pk])
                if ty == 0:
                    P.op("dve", _ts(rt[:], pk[:, 0:383], b31[:, h:h + 1], 8.0, ALU.subtract, ALU.mult),
                         reads=[pk, b31], writes=[rt])
                else:
                    P.op("dve", _stt(rt[:], pk[:, 0:383], 8.0, negc[:, (ty - 1) * 383:ty * 383], ALU.mult, ALU.add),
                         reads=[pk, negc], writes=[rt])
                idx = ty * 8 + h
                scr_w = bass.AP(Rscr_h, idx * 128 * 383, [[383, 128], [1, 383]])
                scr_r = bass.AP(Rscr_h, idx * 128 * 383 + 127, [[382, 128], [1, 256]])
                dRk = T(None, "dRk")
                P.dma("pool", _dma(scr_w, rt[:]), reads=[rt], writes=[dRk])
                P.dma("sp", _dma(nf[:], scr_r), reads=[dRk], writes=[nf])
                if ty == 0:
                    P.op("dve", _cp(NBt[ty][h][:], nf[:]), reads=[nf], writes=[NBt[ty][h]])
                else:
                    eb = EBt[ty - 1][h // 2]
                    P.op("act", _act(eb[:, (h % 2) * 256:(h % 2 + 1) * 256], nf[:], ACTF.Exp, scale=0.125),
                         reads=[nf], pwrites=[eb])
            nb_list = [(ty, h) for ty in range(4) for h in range(8)]

            xs = [sb2(f"xs{j}", [128, 1024], F32) for j in range(2)]
            xn = [sb2(f"xn{j}", [128, 1024], BF16) for j in range(2)]
            junkx = sb2("junkx", [128, 1024], BF16)
            ss = [sb2(f"ss{j}", [128, 1], F32) for j in range(2)]
            sdx = [sb2(f"sdx{j}", [128, 1], F32) for j in range(2)]
            rsx = [sb2(f"rsx{j}", [128, 1], F32) for j in range(2)]
            xnT = [sb2(f"xnT{j}", [128, 8, 512], BF16) for j in range(2)]
            sqb = [sb2(f"sqb{j}", [128, 512], BF16) for j in range(2)]
            sdb = [sb2(f"sdb{j}", [128, 512], F32) for j in range(2)]
            rsb = [sb2(f"rsb{j}", [128, 512], F32) for j in range(2)]
            fst = [sb2(f"fst{j}", [128, 512], BF16) for j in range(3)]
            vst = [sb2(f"vst{j}", [128, 8, 65], BF16) for j in range(2)]
            gst_ = [sb2(f"gst{j}", [128, 512], BF16) for j in range(2)]
            for v_ in vst:
                P.op("dve", lambda e, v_=v_: e.memset(v_[:], 1.0), writes=[v_])
            FM = []
            for j in range(4):
                FM.append((C_QA + 128 * j, 128, QaT_d, dQa, 128 * j, 0))
            for j in range(4):
                FM.append((C_KA + 128 * j, 128, KaT_d, dKa, 128 * j, 1))
            for j in range(2):
                FM.append((C_QI + 128 * j, 128, qiT_d, dqi, 128 * j, None))
            FM.append((C_KI, 128, kiT_d, dki, 0, None))
            for j in range(4):
                FM.append((C_QB + 128 * j, 128, QbT_d, dQb, 128 * j, 2))
            for j in range(4):
                FM.append((C_KB + 128 * j, 128, KbT_d, dKb, 128 * j, 3))
            TM = [(C_VA, "v", Va_d, dVa), (C_ZA, "g", Ga_d, dGa), (C_VB, "v", Vb_d, dVb), (C_ZB, "g", Gb_d, dGb)]
            cn = {"kx": 0, "kf": 0, "kt": 0, "kv": 0, "kg": 0}
            groups = [(b, g) for b in range(NB) for g in range(NG)]

            def prep_norm(q, tt):
                b, g = groups[q]
                t = 4 * g + tt
                kx = q * 4 + tt
                xs_, xn_ = xs[kx % 2], xn[kx % 2]
                ss_, sd_, rs_ = ss[kx % 2], sdx[kx % 2], rsx[kx % 2]
                P.dma("sp", _dma(xs_[:], x_d[b, t * 128:(t + 1) * 128, :]), writes=[xs_])
                P.op("act", _act(junkx[:], xs_[:], ACTF.Square, accum=ss_[:, 0:1]),
                     reads=[xs_], writes=[junkx, ss_])
                P.op("act", _act(sd_[:], ss_[:], ACTF.Ln, bias=epst[:, 0:1], scale=1.0 / D_MODEL),
                     reads=[ss_, epst], writes=[sd_])
                P.op("act", _act(rs_[:], sd_[:], ACTF.Exp, scale=-0.5), reads=[sd_], writes=[rs_])
                P.op("dve", _ts(xn_[:], xs_[:], rs_[:, 0:1], None, ALU.mult), reads=[xs_, rs_], writes=[xn_])

            def prep_tr(q, tt):
                kx = q * 4 + tt
                xn_ = xn[kx % 2]
                xg = xnT[q % 2]
                for c in range(8):
                    P.op("pe", _tr(pT[:, c * 128:(c + 1) * 128], xn_[:, c * 128:(c + 1) * 128], ident[:]),
                         reads=[xn_, ident], **({"writes": [pT]} if c == 0 else {"pwrites": [pT]}))
                P.op("act", _acp(xg[:, :, tt * 128:(tt + 1) * 128], pT[:].rearrange("p (c t) -> p c t", c=8)),
                     reads=[pT], **({"writes": [xg]} if tt == 0 else {"pwrites": [xg]}))

            def fm_item(q, idx):
                b, g = groups[q]
                xg = xnT[q % 2]
                (col0, M, dst, dT_, row0, gc) = FM[idx]
                kf = cn["kf"]
                cn["kf"] += 1
                pf = pb[kf % 2]
                for c in range(8):
                    P.op("pe", _mm(pf[0:M, :], W[:, c, col0:col0 + M], xg[:, c, :], c == 0, c == 7),
                         reads=[W, xg], **({"writes": [pf]} if c == 0 else {"pwrites": [pf]}))
                fs = fst[kf % 3]
                if col0 == C_KI:
                    M = 64
                if gc is not None:
                    sq_, sd2, rs2, pS = sqb[kf % 2], sdb[kf % 2], rsb[kf % 2], pb[2 + kf % 2]
                    P.op("act", _act(sq_[:], pf[:], ACTF.Square), reads=[pf], writes=[sq_])
                    P.op("pe", _mm(pS[:], bones[:], sq_[:], True, True), reads=[bones, sq_], writes=[pS])
                    P.op("act", _act(sd2[:], pS[:], ACTF.Ln, bias=epst[:, 0:1]), reads=[pS, epst], writes=[sd2])
                    P.op("act", _act(rs2[:], sd2[:], ACTF.Exp, scale=-0.5), reads=[sd2], writes=[rs2])
                    P.op("dve", _stt(fs[:], pf[:], qkg[:, gc:gc + 1], rs2[:], ALU.mult, ALU.mult),
                         reads=[pf, qkg, rs2], writes=[fs])
                else:
                    P.op("dve", _cp(fs[0:M, :], pf[0:M, :]), reads=[pf], writes=[fs])
                P.dma("pool", _dma(dst[b, row0:row0 + M, g * 512:(g + 1) * 512], fs[0:M, :]),
                      reads=[fs], pwrites=[dT_])

            def tm_item(q, tt, j):
                b, g = groups[q]
                xg = xnT[q % 2]
                t = 4 * g + tt
                (col0, kind, dst, dT_) = TM[j]
                pt = pb[4 + cn["kt"] % 2]
                cn["kt"] += 1
                for c in range(8):
                    P.op("pe", _mm(pt[:], xg[:, c, tt * 128:(tt + 1) * 128], W[:, c, col0:col0 + 512], c == 0, c == 7),
                         reads=[W, xg], **({"writes": [pt]} if c == 0 else {"pwrites": [pt]}))
                if kind == "v":
                    vs_ = vst[cn["kv"] % 2]
                    cn["kv"] += 1
                    P.op("dve", _cp(vs_[:, :, 0:64], pt[:].rearrange("p (h d) -> p h d", h=8)),
                         reads=[pt], pwrites=[vs_])
                    P.dma("pool", _dma(dst[b, t * 128:(t + 1) * 128, :], vs_[:].rearrange("p h d -> p (h d)")),
                          reads=[vs_], pwrites=[dT_])
                else:
                    gs_ = gst_[cn["kg"] % 2]
                    cn["kg"] += 1
                    P.op("act", _act(gs_[:], pt[:], ACTF.Silu), reads=[pt], writes=[gs_])
                    P.dma("pool", _dma(dst[b, t * 128:(t + 1) * 128, :], gs_[:]), reads=[gs_], pwrites=[dT_])
                if j == 3:
                    pw = pb[6]
                    for c in range(8):
                        P.op("pe", _mm(pw[:, 0:4], xg[:, c, tt * 128:(tt + 1) * 128], W[:, c, C_WI:C_WI + 4], c == 0, c == 7),
                             reads=[W, xg], **({"writes": [pw]} if c == 0 else {"pwrites": [pw]}))
                    P.op("dve", _ts(wi_all[:, b * NT + t, :], pw[:, 0:4], WSCALE, None, ALU.mult),
                         reads=[pw], pwrites=[wi_all])

            for tt in range(4):
                prep_norm(0, tt)
                prep_tr(0, tt)
            nb_b31()
            nbi = 0
            per_g = (len(nb_list) + len(groups) - 1) // len(groups)
            for q in range(len(groups)):
                items = []
                fi, ti = 0, 0
                while fi < len(FM) or ti < 16:
                    if fi < len(FM):
                        items.append(("f", fi))
                        fi += 1
                    if ti < 16:
                        items.append(("t", ti))
                        ti += 1
                for n_, (kind, v) in enumerate(items):
                    if q + 1 < len(groups):
                        if n_ >= 2 and (n_ - 2) % 8 == 0 and (n_ - 2) // 8 < 4:
                            prep_norm(q + 1, (n_ - 2) // 8)
                        if n_ >= 6 and (n_ - 6) % 8 == 0 and (n_ - 6) // 8 < 4:
                            prep_tr(q + 1, (n_ - 6) // 8)
                    if kind == "f":
                        fm_item(q, v)
                    else:
                        tm_item(q, v // 4, v % 4)
                    if n_ in (10, 20, 30)[:per_g + 1] and nbi < len(nb_list):
                        nb_tile(*nb_list[nbi])
                        nbi += 1
            while nbi < len(nb_list):
                nb_tile(*nb_list[nbi])
                nbi += 1
            P.barrier()
            P.flush()

        for b in range(NB):
            with contextlib.ExitStack() as st:
                sb = mk(st)
                KaT = sb("KaT_s", [128, 4, S], BF16)
                VA = sb("VA_s", [128, NT, 520], BF16)
                kiT2 = sb("kiT2", [128, S], BF16)
                scb = [sb(f"sc{j}", [128, S], F32) for j in range(2)]
                nm = [sb(f"nm{j}", [128, S], BF16) for j in range(4)]
                rbuf = [sb(f"rbuf{j}", [128, 512], F32) for j in range(3)]
                pbuf = [sb(f"pbuf{j}", [128, 512], BF16) for j in range(6)]
                plb = [pb[2], pb[3], pb[4], pT]
                Qi = [sb(f"Qi{j}", [128, 8, 128], BF16) for j in range(4)]
                qii = [sb(f"qii{j}", [128, 4, 128], BF16) for j in range(2)]
                Gi = [sb(f"Gi{j}", [128, 8, 64], BF16) for j in range(4)]
                pvs = [sb(f"pvs{j}", [128, 8, 65], F32) for j in range(2)]
                rden = sb("rden", [128, 8, 1], F32)
                t1 = sb("t1", [128, 8, 64], F32)
                mixed = [sb(f"mixed{j}", [128, 8, 64], BF16) for j in range(2)]
                cntb = [sb(f"cnt{j}", [128, 1], F32) for j in range(2)]
                negmb = [sb(f"negm{j}", [128, 1], I32) for j in range(2)]
                candb = [sb(f"cand{j}", [128, 1], I32) for j in range(2)]
                kbb = [sb(f"kb{j}", [128, 1], I32) for j in range(2)]
                P.dma("sp", _dma(KaT[:], KaT_d[b].rearrange("(c p) s -> p c s", p=128)), reads=[dKa], writes=[KaT])
                P.dma("sp", _dma(VA[:], Va_d[b].rearrange("(t p) f -> p t f", p=128)), reads=[dVa], writes=[VA])
                P.dma("sp", _dma(kiT2[0:64, :], kiT_d[b]), reads=[dki], writes=[kiT2])
                P.dma("sp", _dma(kiT2[64:128, :], kiT_d[b]), reads=[dki], pwrites=[kiT2])
                qa_v = QaT_d[b].rearrange("(c hh p) s -> hh p c s", hh=2, p=64)
                qi_v = qiT_d[b].rearrange("(c hh p) s -> hh p c s", hh=2, p=64)
                for z_ in Qi + qii:
                    P.op("pool", lambda e, z_=z_: e.memset(z_[:], 0.0), writes=[z_])
                cnt_sc = [0]
                cnt_l = [0]

                def scores(i):
                    n = 128 * (i + 1)
                    Q_, q_, G_ = Qi[i % 4], qii[i % 2], Gi[i % 4]
                    sc = scb[i % 2]
                    isl = slice(i * 128, (i + 1) * 128)
                    P.dma("sp", _dma(Q_[0:64, 0:8:2, :], qa_v[0][:, :, isl]), reads=[dQa], pwrites=[Q_])
                    P.dma("sp", _dma(Q_[64:128, 1:8:2, :], qa_v[1][:, :, isl]), reads=[dQa], pwrites=[Q_])
                    P.dma("sp", _dma(q_[0:64, 0:4:2, :], qi_v[0][:, :, isl]), reads=[dqi], pwrites=[q_])
                    P.dma("sp", _dma(q_[64:128, 1:4:2, :], qi_v[1][:, :, isl]), reads=[dqi], pwrites=[q_])
                    P.dma("sp", _dma(G_[:].rearrange("p h d -> p (h d)"), Ga_d[b, i * 128:(i + 1) * 128, :]), reads=[dGa], writes=[G_])
                    P.dma("sp", _dma(sc[:, :n], tb_d[:, :n]), writes=[sc])
                    wcol = b * NT + i
                    for kg in range((n + 511) // 512):
                        ncol = min(512, n - kg * 512)
                        cs = slice(kg * 512, kg * 512 + ncol)
                        for h in range(4):
                            pk = pb[cnt_sc[0] % 2]
                            rb = rbuf[cnt_sc[0] % 3]
                            cnt_sc[0] += 1
                            hh, hc = h % 2, h // 2
                            P.op("pe", _mm(pk[:, :ncol], q_[:, h, :], kiT2[:, cs], True, True),
                                 reads=[q_, kiT2], writes=[pk])
                            P.op("act", _act(rb[:, :ncol], pk[:, :ncol], ACTF.Relu), reads=[pk], writes=[rb])
                            P.op("dve", _stt(sc[:, cs], rb[:, :ncol], wi_all[:, wcol, h:h + 1], sc[:, cs], ALU.mult, ALU.add),
                                 reads=[rb, wi_all, sc], pwrites=[sc])
                        yield
                    dsl = slice(i * 128, (i + 1) * 128)
                    P.op("dve", _tt(sc[:, dsl], sc[:, dsl], caus[:], ALU.add), reads=[sc, caus], pwrites=[sc])

                def bisect(blocks):
                    def count(i, mode, s1, rd):
                        n = 128 * (i + 1)
                        sc, cnt, jk = scb[i % 2], cntb[i % 2], nm[i % 4]
                        if mode == "dve":
                            P.op("dve", _ts(jk[:, :n], sc[:, :n], s1, None, ALU.is_ge, ALU.add, accum=cnt[:, 0:1]),
                                 reads=[sc] + rd, writes=[jk, cnt])
                        else:
                            P.op("act", _act(jk[:, :n], sc[:, :n], ACTF.Sign, bias=s1, scale=-1.0, accum=cnt[:, 0:1]),
                                 reads=[sc] + rd, writes=[jk, cnt])

                    def thrc(i, mode):
                        n = 128 * (i + 1)
                        return (KTH, ALU.is_ge, ALU.is_lt) if mode == "dve" else (-(2.0 * TOPK - n - 1.5), ALU.is_le, ALU.is_gt)
                    for (i, mode) in blocks:
                        if mode == "dve":
                            count(i, mode, 0.0, [])
                        else:
                            count(i, mode, zerot[:, 0:1], [zerot])
                    for (i, mode) in blocks:
                        cnt, negm, cand = cntb[i % 2], negmb[i % 2], candb[i % 2]
                        c, opk, opn = thrc(i, mode)
                        P.op("dve", _ts(negm[:], cnt[:], c, -1.0, opn, ALU.mult), reads=[cnt], writes=[negm])
                        P.op("dve", _stt(cand[:], negm[:], bitc[:, 30:31], bitc[:, 29:30], ALU.bitwise_and, ALU.bitwise_xor),
                             reads=[negm, bitc], writes=[cand])
                    yield
                    for bit in range(29, -1, -1):
                        for (i, mode) in blocks:
                            cand = candb[i % 2]
                            count(i, mode, cand[:, 0:1].bitcast(F32), [cand])
                        for (i, mode) in blocks:
                            cnt, cand, kb = cntb[i % 2], candb[i % 2], kbb[i % 2]
                            c, opk, opn = thrc(i, mode)
                            P.op("dve", _ts(kb[:], cnt[:], c, float(2 ** bit), opk, ALU.mult), reads=[cnt], writes=[kb])
                            P.op("dve", _stt(cand[:], kb[:], dbc[:, bit:bit + 1], cand[:], ALU.bitwise_xor, ALU.bitwise_xor),
                                 reads=[kb, dbc, cand], writes=[cand])
                        yield
                    for (i, mode) in blocks:
                        n = 128 * (i + 1)
                        P.op("dve", _ts(nm[i % 4][:, :n], scb[i % 2][:, :n], candb[i % 2][:, 0:1].bitcast(F32), NEGBIG, ALU.is_lt, ALU.mult),
                             reads=[scb[i % 2], candb[i % 2]], writes=[nm[i % 4]])
                    yield

                def stageA(m):
                    blocks = [i for i in (2 * m, 2 * m + 1) if i < NT]
                    for i in blocks:
                        yield from scores(i)
                    modes = ["dve", "act"]
                    yield from bisect([(i, modes[k]) for k, i in enumerate(blocks)])

                def attend(i):
                    Q_, G_ = Qi[i % 4], Gi[i % 4]
                    nm_ = nm[i % 4]
                    pv_ = pvs[i % 2]
                    ngr = (i + 4) // 4
                    steps = [(h, g) for h in range(8) for g in range(ngr)]
                    base = cnt_l[0]
                    cnt_l[0] += len(steps)

                    def plv(k):
                        t_ = plb[(base + k) % 4]
                        return t_, (t_.t[:].bitcast(F32) if t_ is pT else t_.t[:])

                    def qk(k):
                        h, g = steps[k]
                        pl, plap = plv(k)
                        hp, hh = h // 2, h % 2
                        chunks = list(range(4 * g, min(4 * g + 4, i + 1)))
                        for kk, c in enumerate(chunks):
                            o = plap[:, kk * 128:(kk + 1) * 128]
                            near = c >= i - 1
                            P.op("pe", _mm(o, KaT[:, hp, c * 128:(c + 1) * 128], Q_[:, h, :], True, False),
                                 reads=[KaT, Q_], **({"writes": [pl]} if kk == 0 else {"pwrites": [pl]}))
                            P.op("pe", _mm(o, nm_[:, c * 128:(c + 1) * 128], ident[:], False, not near),
                                 reads=[nm_, ident], pwrites=[pl])
                            if near:
                                nbs = NBt[0][h][:, 0:128] if c == i else NBt[0][h][:, 128:256]
                                P.op("pe", _mm(o, ident[:], nbs, False, True), reads=[NBt[0][h], ident], pwrites=[pl])

                    def ex(k):
                        h, g = steps[k]
                        pl, plap = plv(k)
                        pbf = pbuf[(base + k) % 6]
                        ncol = 128 * (min(4 * g + 4, i + 1) - 4 * g)
                        P.op("act", _act(pbf[:, :ncol], plap[:, :ncol], ACTF.Exp, bias=b31[:, h:h + 1], scale=0.125),
                             reads=[pl, b31], writes=[pbf])

                    def pv(k):
                        h, g = steps[k]
                        pbf = pbuf[(base + k) % 6]
                        pp = pb[5 + h % 2]
                        chunks = list(range(4 * g, min(4 * g + 4, i + 1)))
                        for kk, c in enumerate(chunks):
                            P.op("pe", _mm(pp[:, 0:65], pbf[:, kk * 128:(kk + 1) * 128], VA[:, c, h * 65:(h + 1) * 65], c == 0, c == i),
                                 reads=[pbf, VA], **({"writes": [pp]} if c == 0 else {"pwrites": [pp]}))
                        if g == ngr - 1:
                            P.op("act", _acp(pv_[:, h, :], pp[:, 0:65]), reads=[pp],
                                 **({"writes": [pv_]} if h == 0 else {"pwrites": [pv_]}))
                    qk(0)
                    if len(steps) > 1:
                        qk(1)
                    for k in range(len(steps)):
                        if k + 2 < len(steps):
                            qk(k + 2)
                        ex(k)
                        pv(k)
                        yield
                    mx = mixed[i % 2]
                    P.op("dve", lambda e: e.reciprocal(rden[:], pv_[:, :, 64:65]), reads=[pv_], writes=[rden])
                    P.op("dve", _tt(t1[:], pv_[:, :, 0:64], rden[:].to_broadcast([128, 8, 64]), ALU.mult), reads=[pv_, rden], writes=[t1])
                    P.op("dve", _tt(mx[:], t1[:], G_[:], ALU.mult), reads=[t1, G_], writes=[mx])
                    P.dma("pool", _dma(MixA_d[b, i * 128:(i + 1) * 128, :], mx[:].rearrange("p h d -> p (h d)")),
                          reads=[mx], pwrites=[dMix])
                    yield

                def stageB(m):
                    for i in (2 * m, 2 * m + 1):
                        if i < NT:
                            yield from attend(i)

                NP_ = (NT + 1) // 2

                def unitsA(m):
                    u = 0
                    for i in (2 * m, 2 * m + 1):
                        if i < NT:
                            u += (128 * (i + 1) + 511) // 512
                    return u + 32

                def unitsB(m):
                    u = 0
                    for i in (2 * m, 2 * m + 1):
                        if i < NT:
                            u += 8 * ((i + 4) // 4) + 1
                    return u
                for _ in stageA(0):
                    pass
                for m in range(NP_):
                    ga = stageA(m + 1) if m + 1 < NP_ else iter(())
                    gbb = stageB(m)
                    ua = unitsA(m + 1) if m + 1 < NP_ else 0
                    ub = unitsB(m)
                    da = db = 0
                    a_alive, b_alive = ua > 0, True
                    while a_alive or b_alive:
                        if a_alive and (not b_alive or da * ub <= db * ua):
                            try:
                                next(ga)
                                da += 1
                            except StopIteration:
                                a_alive = False
                        else:
                            try:
                                next(gbb)
                                db += 1
                            except StopIteration:
                                b_alive = False
                P.barrier()
                P.flush()

            with contextlib.ExitStack() as st:
                sb = mk(st)
                QbT = sb("QbT_s", [128, 8, S], BF16)
                KbT = sb("KbT_s", [128, 4, S], BF16)
                vt = [sb(f"vt{j}", [128, 520], BF16) for j in range(3)]
                pbuf = [sb(f"pbufb{j}", [128, 512], BF16) for j in range(5)]
                plbB = [pb[0], pb[1], pb[2], pT]
                res = [sb(f"res{j}", [128, 520], F32) for j in range(2)]
                P.op("pool", lambda e: e.memset(QbT[:], 0.0), writes=[QbT])
                qb_v = QbT_d[b].rearrange("(c hh p) s -> hh p c s", hh=2, p=64)
                P.dma("sp", _dma(QbT[0:64, 0:8:2, :], qb_v[0]), reads=[dQb], pwrites=[QbT])
                P.dma("sp", _dma(QbT[64:128, 1:8:2, :], qb_v[1]), reads=[dQb], pwrites=[QbT])
                P.dma("sp", _dma(KbT[:], KbT_d[b].rearrange("(c p) s -> p c s", p=128)), reads=[dKb], writes=[KbT])
                tiles = []
                for p_, dil in enumerate(DILS):
                    nbk = S // (dil * 128)
                    for r in range(dil):
                        for c in range(nbk):
                            tiles.append((p_, dil, r, c))
                steps = [(ti, hp) for ti in range(len(tiles)) for hp in range(4)]

                def qsl(dil, r, c):
                    return slice(r + 128 * c * dil, r + 128 * c * dil + 127 * dil + 1, dil)

                def qkB(k):
                    ti, hp = steps[k]
                    p_, dil, r, c = tiles[ti]
                    qs = qsl(dil, r, c)
                    if hp == 0:
                        v_ = vt[ti % 3]
                        P.dma("sp", _dma(v_[:], Vb_d[b, qs, :]), reads=[dVb], writes=[v_])
                    pl = plbB[k % 4]
                    plap = pl.t[:].bitcast(F32) if pl is pT else pl.t[:]
                    firstw = True
                    for hh in range(2):
                        h = 2 * hp + hh
                        bs = hh * 256
                        srcs = [(0, qs)] + ([(128, qsl(dil, r, c - 1))] if c >= 1 else [])
                        for off, ks in srcs:
                            o = plap[:, bs + off:bs + off + 128]
                            P.op("pe", _mm(o, KbT[:, hp, ks], QbT[:, h, qs], True, True),
                                 reads=[KbT, QbT], **({"writes": [pl]} if firstw else {"pwrites": [pl]}))
                            firstw = False

                def exB(k):
                    ti, hp = steps[k]
                    p_, dil, r, c = tiles[ti]
                    pl = plbB[k % 4]
                    plap = pl.t[:].bitcast(F32) if pl is pT else pl.t[:]
                    pbf = pbuf[k % 5]
                    eb = EBt[p_][hp]
                    if c >= 1:
                        P.op("act", _act(pbf[:], plap, ACTF.Exp, scale=0.125), reads=[pl], writes=[pbf])
                        P.op("dve", _tt(pbf[:], pbf[:], eb[:], ALU.mult), reads=[pbf, eb], writes=[pbf])
                    else:
                        v3 = lambda t_: t_[:].rearrange("p (a b) -> p a b", a=2)[:, :, 0:128]
                        P.op("act", _act(v3(pbf), plap.rearrange("p (a b) -> p a b", a=2)[:, :, 0:128], ACTF.Exp, scale=0.125), reads=[pl], writes=[pbf])
                        P.op("dve", _tt(v3(pbf), v3(pbf), v3(eb), ALU.mult), reads=[pbf, eb], writes=[pbf])

                def pvB(k):
                    ti, hp = steps[k]
                    p_, dil, r, c = tiles[ti]
                    pbf = pbuf[k % 5]
                    v_ = vt[ti % 3]
                    vp = vt[(ti - 1) % 3]
                    for hh in range(2):
                        h = 2 * hp + hh
                        bs = hh * 256
                        pp = pb[3 + 2 * (ti % 2) + h // 4]
                        col = (h % 4) * 65
                        P.op("pe", _mm(pp[:, col:col + 65], pbf[:, bs:bs + 128], v_[:, h * 65:(h + 1) * 65], True, c == 0),
                             reads=[pbf, v_], **({"writes": [pp]} if h % 4 == 0 else {"pwrites": [pp]}))
                        if c >= 1:
                            P.op("pe", _mm(pp[:, col:col + 65], pbf[:, bs + 128:bs + 256], vp[:, h * 65:(h + 1) * 65], False, True),
                                 reads=[pbf, vp], pwrites=[pp])
                    if hp == 3:
                        rs_ = res[ti % 2]
                        P.op("act", _acp(rs_[:, 0:260], pb[3 + 2 * (ti % 2)][:, 0:260]), reads=[pb[3 + 2 * (ti % 2)]], writes=[rs_])
                        P.op("dve", _cp(rs_[:, 260:520], pb[4 + 2 * (ti % 2)][:, 0:260]), reads=[pb[4 + 2 * (ti % 2)]], pwrites=[rs_])
                        P.dma("pool", _dma(Bres_d[b, p_, qsl(dil, r, c), :], rs_[:]), reads=[rs_], pwrites=[dBres])
                nst = len(steps)
                for k0 in range(min(3, nst)):
                    qkB(k0)
                exB(0)
                for k in range(nst):
                    if k + 3 < nst:
                        qkB(k + 3)
                    if k + 1 < nst:
                        exB(k + 1)
                    pvB(k)
                P.barrier()
                P.flush()

            with contextlib.ExitStack() as st:
                sb = mk(st)
                xs = [sb(f"xf{j}", [128, 1024], F32) for j in range(3)]
                mix = [sb(f"mix{j}", [128, 1024], BF16) for j in range(3)]
                rr = [[sb(f"rr{j}_{q}", [128, 8, 65], F32) for q in range(3)] for j in range(3)]
                gb = [sb(f"gb{j}", [128, 8, 64], BF16) for j in range(3)]
                ssum = [sb(f"ssum{j}", [128, 8, 65], F32) for j in range(2)]
                rden = [sb(f"rdenf{j}", [128, 8, 1], F32) for j in range(2)]
                t1 = [sb(f"t1f{j}", [128, 8, 64], F32) for j in range(2)]
                mixT = [sb(f"mixT{j}", [128, 1024], BF16) for j in range(2)]
                ot = [sb(f"ot{j}", [128, 1024], F32) for j in range(2)]

                def FL(t):
                    j = t % 3
                    rows = slice(t * 128, (t + 1) * 128)
                    P.dma("sp", _dma(xs[j][:], x_d[b, rows, :]), writes=[xs[j]])
                    P.dma("sp", _dma(mix[j][:, 0:512], MixA_d[b, rows, :]), reads=[dMix], writes=[mix[j]])
                    for q in range(3):
                        P.dma("sp", _dma(rr[j][q][:].rearrange("p h d -> p (h d)"), Bres_d[b, q, rows, :]), reads=[dBres], writes=[rr[j][q]])
                    P.dma("sp", _dma(gb[j][:].rearrange("p h d -> p (h d)"), Gb_d[b, rows, :]), reads=[dGb], writes=[gb[j]])

                def F1(t):
                    j = t % 3
                    sm, rd, tt1 = ssum[t % 2], rden[t % 2], t1[t % 2]
                    P.op("dve", _tt(sm[:], rr[j][0][:], rr[j][1][:], ALU.add), reads=[rr[j][0], rr[j][1]], writes=[sm])
                    P.op("dve", _tt(sm[:], sm[:], rr[j][2][:], ALU.add), reads=[sm, rr[j][2]], writes=[sm])
                    P.op("dve", lambda e: e.reciprocal(rd[:], sm[:, :, 64:65]), reads=[sm], writes=[rd])
                    P.op("dve", _tt(tt1[:], sm[:, :, 0:64], rd[:].to_broadcast([128, 8, 64]), ALU.mult), reads=[sm, rd], writes=[tt1])
                    P.op("dve", _tt(mix[j][:, 512:1024].rearrange("p (h d) -> p h d", h=8), tt1[:], gb[j][:], ALU.mult),
                         reads=[tt1, gb[j]], pwrites=[mix[j]])

                def F2(t):
                    j = t % 3
                    j2 = t % 2
                    rows = slice(t * 128, (t + 1) * 128)
                    for c in range(8):
                        P.op("pe", _tr(pT[:, c * 128:(c + 1) * 128], mix[j][:, c * 128:(c + 1) * 128], ident[:]),
                             reads=[mix[j], ident], **({"writes": [pT]} if c == 0 else {"pwrites": [pT]}))
                    P.op("act", _acp(mixT[j2][:], pT[:]), reads=[pT], writes=[mixT[j2]])
                    for half in range(2):
                        po = pb[2 * j2 + half]
                        for c in range(8):
                            P.op("pe", _mm(po[:], mixT[j2][:, c * 128:(c + 1) * 128], Wo[:, c, half * 512:(half + 1) * 512], c == 0, c == 7),
                                 reads=[mixT[j2], Wo], **({"writes": [po]} if c == 0 else {"pwrites": [po]}))
                        P.op("dve", _tt(ot[j2][:, half * 512:(half + 1) * 512], po[:], xs[j][:, half * 512:(half + 1) * 512], ALU.add),
                             reads=[po, xs[j]], **({"writes": [ot[j2]]} if half == 0 else {"pwrites": [ot[j2]]}))
                    tok = P.dma("pool", _dma(out_d[b, rows, :], ot[j2][:]), reads=[ot[j2]])
                    out_tokens[tok[0]] = max(out_tokens.get(tok[0], 0), tok[1])
                FL(0)
                if NT > 1:
                    FL(1)
                F1(0)
                for t in range(NT):
                    if t + 2 < NT:
                        FL(t + 2)
                    if t + 1 < NT:
                        F1(t + 1)
                    F2(t)
                P.barrier()
                P.flush(final_tokens=out_tokens if b == NB - 1 else None)
    return nc


def _rel_bucket(d):
    d = np.maximum(np.asarray(d, np.int64), 0)
    df = np.maximum(d, 1).astype(np.float32)
    large = 16 + (np.log(df / np.float32(16)) / np.float32(math.log(128 / 16)) * np.float32(16)).astype(np.int32)
    large = np.minimum(large, 31)
    return np.where(d < 16, d, large).astype(np.int64)


def host_consts(S):
    bf = ml_dtypes.bfloat16
    c = {}
    c["ident"] = np.eye(128, dtype=np.float32).astype(bf)
    bo = np.zeros((128, 128), np.float32)
    bo[:64, :64] = 1.0 / 64
    bo[64:, 64:] = 1.0 / 64
    c["bones"] = bo.astype(bf)
    q = np.arange(128)[:, None]
    s = np.arange(128)[None, :]
    c["caus"] = np.where(s <= q, 0.0, -4.0).astype(np.float32)
    c["tb"] = np.tile((-(np.arange(S, dtype=np.float64) + 1) * 2.0 ** -100).astype(np.float32)[None], (128, 1))
    oh = np.zeros((32, 4 * 383), np.float32)
    neg = np.zeros((128, 3 * 383), np.float32)
    xs = np.arange(383)
    d = xs - 127
    bk = _rel_bucket(d)
    for x_ in range(383):
        if d[x_] >= 0:
            oh[bk[x_], x_] = 1.0
    for p_, dil in enumerate(DILS):
        valid = (d >= 0) & (d <= 128)
        bkp = _rel_bucket(d * dil)
        for x_ in range(383):
            if valid[x_]:
                oh[bkp[x_], (1 + p_) * 383 + x_] = 1.0
            else:
                neg[:, p_ * 383 + x_] = 8.0 * NEGBIG
    c["oh"] = oh
    c["negc"] = neg
    o31 = np.zeros((32, 128), np.float32)
    o31[31, :] = 1.0
    c["oh31"] = o31
    bc = np.zeros((128, 32), np.int32)
    for b in range(30):
        bc[:, b] = 1 << b
    bc[:, 30] = np.int32(-1073741825)
    c["bitc"] = bc
    db = np.zeros((128, 32), np.int32)
    db[:, 0] = 1
    for b in range(1, 30):
        db[:, b] = (1 << b) ^ (1 << (b - 1))
    c["dbc"] = db
    return c


def host_inputs(S, x_c, norm_gain, w_in, w_out, rel_bias, q_norm_a, k_norm_a, q_norm_b, k_norm_b):
    m = dict(host_consts(S))
    m["x"] = np.ascontiguousarray(x_c, dtype=np.float32)
    m["w_in"] = np.ascontiguousarray(w_in[0], dtype=np.float32)
    m["w_out"] = np.ascontiguousarray(w_out[0], dtype=np.float32)
    m["gain8"] = np.ascontiguousarray(np.asarray(norm_gain[0], np.float32).reshape(8, 128).T)
    m["relb"] = np.ascontiguousarray(rel_bias, dtype=np.float32)
    m["qkg"] = np.ascontiguousarray(np.stack(
        [np.tile(np.asarray(g[0], np.float32), 2) for g in (q_norm_a, k_norm_a, q_norm_b, k_norm_b)], axis=1))
    return m


_NC_CACHE = {}


def kernel(x, norm_gain, w_in, w_out, rel_bias, q_norm_a, k_norm_a, q_norm_b, k_norm_b):
    x = np.asarray(x)
    B, S, D = x.shape
    n = 8
    NB = B // n
    key = (S, NB)
    if key not in _NC_CACHE:
        _NC_CACHE[key] = build(S, NB)
    nc = _NC_CACHE[key]
    args = [np.asarray(a) for a in (norm_gain, w_in, w_out, rel_bias, q_norm_a, k_norm_a, q_norm_b, k_norm_b)]
    in_maps = [host_inputs(S, x[k * NB:(k + 1) * NB], *args) for k in range(n)]
    res = run_bass_kernel_spmd(nc, in_maps, core_ids=list(range(n)))
    return np.concatenate([r["out"] for r in res.results], axis=0).astype(np.float32)
```

```python
import math
import contextlib
import numpy as np
import ml_dtypes
import concourse.bass as bass
import concourse.mybir as mybir
from concourse.bass_utils import run_bass_kernel_spmd

F32 = mybir.dt.float32
BF16 = mybir.dt.bfloat16
I32 = mybir.dt.int32
ALU = mybir.AluOpType
ACTF = mybir.ActivationFunctionType

NDMA_SEM = 12
D_MODEL = 1024
D_IN = 4420
EPS = 1e-6
C_QA, C_KA, C_VA, C_ZA, C_QI, C_KI, C_WI, C_QB, C_KB, C_VB, C_ZB = (
    0, 512, 1024, 1536, 2048, 2304, 2368, 2372, 2884, 3396, 3908)
DILS = (1, 4, 16)
NEGBIG = -30000.0
WSCALE = 2.0 ** -12


class T:
    __slots__ = ("t", "writers", "readers", "name", "full")

    def __init__(self, t, name=""):
        self.t = t
        self.writers = []
        self.readers = []
        self.name = name
        self.full = None

    def __getitem__(self, k):
        return self.t[k]


class Prog:
    ENGS = ("pe", "act", "dve", "pool", "sp")

    def __init__(self, nc, st):
        self.nc = nc
        self.ops = {e: [] for e in self.ENGS}
        self.count = {e: 0 for e in self.ENGS}
        self.seen = {e: {} for e in self.ENGS}
        self.dma_n = {"sp": 0, "pool": 0}
        self.dma_last = {}
        self.pending = {e: [] for e in self.ENGS}
        self.n_instr = 0
        self.sems = {}
        for e in ("pe", "act", "dve", "pool"):
            self.sems[e] = st.enter_context(nc.semaphore("s_" + e))
        for q in ("sp", "pool"):
            for j in range(NDMA_SEM):
                self.sems[("dma", q, j)] = st.enter_context(nc.semaphore(f"d_{q}{j}"))

    def _deps(self, eng, reads, writes, pwrites):
        deps = {}

        def add(tok):
            k, v = tok
            if deps.get(k, 0) < v:
                deps[k] = v
        for t in reads:
            for w in t.writers:
                add(w)
        for t in writes:
            for w in t.writers:
                add(w)
            for r in t.readers:
                add(r)
        for t in pwrites:
            for r in t.readers:
                add(r)
            if t.full is not None:
                add(t.full)
        if self.pending[eng]:
            for tok in self.pending[eng]:
                add(tok)
            self.pending[eng] = []
        return deps

    def _commit(self, tok, reads, writes, pwrites):
        for t in reads:
            t.readers.append(tok)
        for t in writes:
            t.writers = [tok]
            t.readers = []
            t.full = tok
        for t in pwrites:
            t.writers.append(tok)

    def _waits(self, eng, deps):
        ws = []
        seen = self.seen[eng]
        for k, v in deps.items():
            if k == "pe" and eng == "pe":
                continue
            if seen.get(k, 0) < v:
                seen[k] = v
                ws.append((k, v))
        return ws

    def op(self, eng, fn, reads=(), writes=(), pwrites=()):
        deps = self._deps(eng, reads, writes, pwrites)
        ws = self._waits(eng, deps)
        self.count[eng] += 1
        tok = (eng, self.count[eng])
        self.ops[eng].append((1, fn, ws, tok))
        self._commit(tok, reads, writes, pwrites)
        self.n_instr += 1
        return tok

    def dma(self, q, fn, reads=(), writes=(), pwrites=()):
        deps = self._deps(q, reads, writes, pwrites)
        n = self.dma_n[q]
        self.dma_n[q] = n + 1
        key = ("dma", q, n % NDMA_SEM)
        val = 16 * (n // NDMA_SEM + 1)
        if n >= NDMA_SEM and deps.get(key, 0) < val - 16:
            deps[key] = val - 16
        ws = self._waits(q, deps)
        tok = (key, val)
        self.dma_last[key] = val
        self.ops[q].append((16, fn, ws, tok))
        self._commit(tok, reads, writes, pwrites)
        self.n_instr += 1
        return tok

    def barrier(self):
        toks = [(e, self.count[e]) for e in ("pe", "act", "dve", "pool") if self.count[e]]
        toks += list(self.dma_last.items())
        for e in self.ENGS:
            self.pending[e] = list(toks)

    def flush(self, final_tokens=None):
        nc = self.nc
        sems = self.sems
        ops = self.ops
        self.ops = {e: [] for e in self.ENGS}

        def run(name, eng):
            for inc, fn, ws, tok in ops[name]:
                for k, v in ws:
                    eng.wait_ge(sems[k], v)
                fn(eng).then_inc(sems[tok[0]], inc)
            if name == "sp" and final_tokens:
                for k, v in final_tokens.items():
                    eng.wait_ge(sems[k], v)

        with nc.Block() as block:
            @block.tensor
            def _(e):
                run("pe", e)

            @block.scalar
            def _(e):
                run("act", e)

            @block.vector
            def _(e):
                run("dve", e)

            @block.gpsimd
            def _(e):
                run("pool", e)

            @block.sync
            def _(e):
                run("sp", e)


def _mm(o, l, r, start, stop):
    return lambda e: e.matmul(o, lhsT=l, rhs=r, start=start, stop=stop)


def _act(o, i, func, bias=None, scale=1.0, accum=None):
    kw = {}
    if bias is not None:
        kw["bias"] = bias
    if accum is not None:
        kw["accum_out"] = accum
    return lambda e: e.activation(out=o, in_=i, func=func, scale=scale, **kw)


def _ts(o, i, s1, s2, op0, op1=None, accum=None):
    kw = {}
    if op1 is not None:
        kw["op1"] = op1
    if accum is not None:
        kw["accum_out"] = accum
    return lambda e: e.tensor_scalar(out=o, in0=i, scalar1=s1, scalar2=s2, op0=op0, **kw)


def _tt(o, a, b, op):
    return lambda e: e.tensor_tensor(out=o, in0=a, in1=b, op=op)


def _stt(o, a, s, b, op0, op1):
    return lambda e: e.scalar_tensor_tensor(out=o, in0=a, scalar=s, in1=b, op0=op0, op1=op1)


def _cp(o, i):
    return lambda e: e.tensor_copy(out=o, in_=i)


def _acp(o, i):
    return lambda e: e.activation(out=o, in_=i, func=ACTF.Copy)


def _dma(o, i):
    return lambda e: e.dma_start(out=o, in_=i)


def _tr(o, i, ident):
    return lambda e: e.transpose(o, i, ident)


def build(S, NB):
    TOPK = min(256, S // 4)
    KTH = float(TOPK) - 0.5
    NT = S // 128
    NG = S // 512
    nc = bass.Bass("TRN2", target_bir_lowering=False)

    def din(name, shape, dt):
        return nc.dram_tensor(name, shape, dt, kind="ExternalInput").ap()

    def dscr(name, shape, dt):
        return nc.dram_tensor(name, shape, dt, kind="Internal")

    x_d = din("x", [NB, S, D_MODEL], F32)
    win_d = din("w_in", [D_MODEL, D_IN], F32)
    wout_d = din("w_out", [D_MODEL, D_MODEL], F32)
    gain8_d = din("gain8", [128, 8], F32)
    relb_d = din("relb", [32, 16], F32)
    qkg_d = din("qkg", [128, 4], F32)
    ident_d = din("ident", [128, 128], BF16)
    bones_d = din("bones", [128, 128], BF16)
    caus_d = din("caus", [128, 128], F32)
    tb_d = din("tb", [128, S], F32)
    oh_d = din("oh", [32, 4 * 383], F32)
    neg_d = din("negc", [128, 3 * 383], F32)
    oh31_d = din("oh31", [32, 128], F32)
    bitc_d = din("bitc", [128, 32], I32)
    dbc_d = din("dbc", [128, 32], I32)
    out_d = nc.dram_tensor("out", [NB, S, D_MODEL], F32, kind="ExternalOutput").ap()

    QaT_d = dscr("QaT", [NB, 512, S], BF16).ap()
    KaT_d = dscr("KaT", [NB, 512, S], BF16).ap()
    QbT_d = dscr("QbT", [NB, 512, S], BF16).ap()
    KbT_d = dscr("KbT", [NB, 512, S], BF16).ap()
    qiT_d = dscr("qiT", [NB, 256, S], BF16).ap()
    kiT_d = dscr("kiT", [NB, 64, S], BF16).ap()
    Va_d = dscr("Va", [NB, S, 520], BF16).ap()
    Vb_d = dscr("Vb", [NB, S, 520], BF16).ap()
    Ga_d = dscr("Ga", [NB, S, 512], BF16).ap()
    Gb_d = dscr("Gb", [NB, S, 512], BF16).ap()
    MixA_d = dscr("MixA", [NB, S, 512], BF16).ap()
    Bres_d = dscr("Bres", [NB, 3, S, 520], F32).ap()
    Rscr_h = dscr("Rscr", [32 * 128 * 383], F32)
    dQa, dKa, dQb, dKb, dqi, dki = [T(None, n) for n in ("dQa", "dKa", "dQb", "dKb", "dqi", "dki")]
    dVa, dVb, dGa, dGb, dMix, dBres, dR = [T(None, n) for n in ("dVa", "dVb", "dGa", "dGb", "dMix", "dBres", "dR")]

    out_tokens = {}

    with contextlib.ExitStack() as gst:
        P = Prog(nc, gst)

        uniq = [0]

        def mk(st):
            def sb(name, shape, dt):
                uniq[0] += 1
                return T(st.enter_context(nc.sbuf_tensor(f"{name}_u{uniq[0]}", shape, dt)), name)
            return sb
        gsb = mk(gst)
        pb = [T(gst.enter_context(nc.psum_tensor(f"pb{j}", [128, 512], F32)), f"pb{j}") for j in range(7)]
        pT = T(gst.enter_context(nc.psum_tensor("pT", [128, 1024], BF16)), "pT")

        ident = gsb("ident", [128, 128], BF16)
        bones = gsb("bones", [128, 128], BF16)
        caus = gsb("caus", [128, 128], F32)
        bitc = gsb("bitc", [128, 32], I32)
        dbc = gsb("dbc", [128, 32], I32)
        zerot = gsb("zerot", [128, 1], F32)
        gain8 = gsb("gain8", [128, 8], F32)
        qkg = gsb("qkg", [128, 4], F32)
        epst = gsb("epst", [128, 1], F32)
        b31 = gsb("b31", [128, 16], F32)
        wi_all = gsb("wi_all", [128, NB * NT, 4], F32)
        Wo = gsb("Wo", [128, 8, 1024], BF16)
        NBt = [[gsb(f"NB{ty}_{h}", [128, 256], BF16) for h in range(8)] for ty in range(1)]
        EBt = [[gsb(f"EB{p_}_{hp}", [128, 512], BF16) for hp in range(4)] for p_ in range(3)]
        for t_, d_ in ((ident, ident_d), (bones, bones_d), (caus, caus_d), (bitc, bitc_d), (dbc, dbc_d),
                       (gain8, gain8_d), (qkg, qkg_d)):
            P.dma("sp", _dma(t_[:], d_), writes=[t_])
        P.op("dve", lambda e: e.memset(epst[:], EPS), writes=[epst])
        P.op("dve", lambda e: e.memset(zerot[:], 0.0), writes=[zerot])

        with contextlib.ExitStack() as st:
            sb = mk(st)
            sb2 = sb
            relb = sb("relb", [32, 16], F32)
            oh = sb("oh", [32, 4 * 383], F32)
            negc = sb("negc", [128, 3 * 383], F32)
            oh31 = sb("oh31", [32, 128], F32)
            rep = [sb(f"rep{j}", [32, 128], F32) for j in range(2)]
            Rt = [sb(f"Rt{j}", [128, 383], F32) for j in range(2)]
            NBf = [sb(f"NBf{j}", [128, 256], F32) for j in range(2)]
            wst = [sb(f"wst{j}", [128, 8, 512], F32) for j in range(2)]
            W = sb2("W", [128, 8, D_IN], BF16)
            wi_v = win_d.rearrange("(c p) n -> p c n", p=128)
            k = 0
            for c0 in range(0, D_IN, 512):
                nco = min(512, D_IN - c0)
                ws_ = wst[k % 2]
                P.dma("sp", _dma(ws_[:, :, :nco], wi_v[:, :, c0:c0 + nco]), writes=[ws_])
                for c in range(8):
                    P.op("dve" if c % 2 == 0 else "pool", _ts(W[:, c, c0:c0 + nco], ws_[:, c, :nco], gain8[:, c:c + 1], 1.0, ALU.mult, ALU.mult),
                         reads=[ws_, gain8], pwrites=[W])
                k += 1
            wo_v = wout_d.rearrange("(c p) n -> p c n", p=128)
            for j in range(2):
                ws_ = wst[k % 2]
                k += 1
                P.dma("sp", _dma(ws_[:], wo_v[:, :, j * 512:(j + 1) * 512]), writes=[ws_])
                for c in range(8):
                    P.op("dve" if c % 2 == 0 else "pool", _cp(Wo[:, c, j * 512:(j + 1) * 512], ws_[:, c, :]), reads=[ws_], pwrites=[Wo])
            for t_, d_ in ((relb, relb_d), (oh, oh_d), (negc, neg_d), (oh31, oh31_d)):
                P.dma("sp", _dma(t_[:], d_), writes=[t_])

            nbk = [0]

            def nb_b31():
                P.op("pe", _mm(pb[6][:, 0:16], oh31[:], relb[:], True, True), reads=[oh31, relb], writes=[pb[6]])
                P.op("dve", _cp(b31[:], pb[6][:, 0:16]), reads=[pb[6]], writes=[b31])

            def nb_tile(ty, h):
                k = nbk[0]
                nbk[0] += 1
                col = h if ty == 0 else 8 + h
                rp = rep[k % 2]
                rt = Rt[k % 2]
                nf = NBf[k % 2]
                pk = pb[6]
                P.op("dve", _cp(rp[:], relb[:, col:col + 1].to_broadcast([32, 128])), reads=[relb], writes=[rp])
                P.op("pe", _mm(pk[:, 0:383], rp[:], oh[:, ty * 383:(ty + 1) * 383], True, True),
                     reads=[rp, oh], writes=[pk])
                if ty == 0:
                    P.op("dve", _ts(rt[:], pk[:, 0:383], b31[:, h:h + 1], 8.0, ALU.subtract, ALU.mult),
                         reads=[pk, b31], writes=[rt])
                else:
                    P.op("dve", _stt(rt[:], pk[:, 0:383], 8.0, negc[:, (ty - 1) * 383:ty * 383], ALU.mult, ALU.add),
                         reads=[pk, negc], writes=[rt])
                idx = ty * 8 + h
                scr_w = bass.AP(Rscr_h, idx * 128 * 383, [[383, 128], [1, 383]])
                scr_r = bass.AP(Rscr_h, idx * 128 * 383 + 127, [[382, 128], [1, 256]])
                dRk = T(None, "dRk")
                P.dma("pool", _dma(scr_w, rt[:]), reads=[rt], writes=[dRk])
                P.dma("sp", _dma(nf[:], scr_r), reads=[dRk], writes=[nf])
                if ty == 0:
                    P.op("dve", _cp(NBt[ty][h][:], nf[:]), reads=[nf], writes=[NBt[ty][h]])
                else:
                    eb = EBt[ty - 1][h // 2]
                    P.op("act", _act(eb[:, (h % 2) * 256:(h % 2 + 1) * 256], nf[:], ACTF.Exp, scale=0.125),
                         reads=[nf], pwrites=[eb])
            nb_list = [(ty, h) for ty in range(4) for h in range(8)]

            xs = [sb2(f"xs{j}", [128, 1024], F32) for j in range(2)]
            xn = [sb2(f"xn{j}", [128, 1024], BF16) for j in range(2)]
            junkx = sb2("junkx", [128, 1024], BF16)
            ss = [sb2(f"ss{j}", [128, 1], F32) for j in range(2)]
            sdx = [sb2(f"sdx{j}", [128, 1], F32) for j in range(2)]
            rsx = [sb2(f"rsx{j}", [128, 1], F32) for j in range(2)]
            xnT = [sb2(f"xnT{j}", [128, 8, 512], BF16) for j in range(2)]
            sqb = [sb2(f"sqb{j}", [128, 512], BF16) for j in range(2)]
            sdb = [sb2(f"sdb{j}", [128, 512], F32) for j in range(2)]
            rsb = [sb2(f"rsb{j}", [128, 512], F32) for j in range(2)]
            fst = [sb2(f"fst{j}", [128, 512], BF16) for j in range(3)]
            vst = [sb2(f"vst{j}", [128, 8, 65], BF16) for j in range(2)]
            gst_ = [sb2(f"gst{j}", [128, 512], BF16) for j in range(2)]
            for v_ in vst:
                P.op("dve", lambda e, v_=v_: e.memset(v_[:], 1.0), writes=[v_])
            FM = []
            for j in range(4):
                FM.append((C_QA + 128 * j, 128, QaT_d, dQa, 128 * j, 0))
            for j in range(4):
                FM.append((C_KA + 128 * j, 128, KaT_d, dKa, 128 * j, 1))
            for j in range(2):
                FM.append((C_QI + 128 * j, 128, qiT_d, dqi, 128 * j, None))
            FM.append((C_KI, 128, kiT_d, dki, 0, None))
            for j in range(4):
                FM.append((C_QB + 128 * j, 128, QbT_d, dQb, 128 * j, 2))
            for j in range(4):
                FM.append((C_KB + 128 * j, 128, KbT_d, dKb, 128 * j, 3))
            TM = [(C_VA, "v", Va_d, dVa), (C_ZA, "g", Ga_d, dGa), (C_VB, "v", Vb_d, dVb), (C_ZB, "g", Gb_d, dGb)]
            cn = {"kx": 0, "kf": 0, "kt": 0, "kv": 0, "kg": 0}
            groups = [(b, g) for b in range(NB) for g in range(NG)]

            def prep_norm(q, tt):
                b, g = groups[q]
                t = 4 * g + tt
                kx = q * 4 + tt
                xs_, xn_ = xs[kx % 2], xn[kx % 2]
                ss_, sd_, rs_ = ss[kx % 2], sdx[kx % 2], rsx[kx % 2]
                P.dma("sp", _dma(xs_[:], x_d[b, t * 128:(t + 1) * 128, :]), writes=[xs_])
                P.op("act", _act(junkx[:], xs_[:], ACTF.Square, accum=ss_[:, 0:1]),
                     reads=[xs_], writes=[junkx, ss_])
                P.op("act", _act(sd_[:], ss_[:], ACTF.Ln, bias=epst[:, 0:1], scale=1.0 / D_MODEL),
                     reads=[ss_, epst], writes=[sd_])
                P.op("act", _act(rs_[:], sd_[:], ACTF.Exp, scale=-0.5), reads=[sd_], writes=[rs_])
                P.op("dve", _ts(xn_[:], xs_[:], rs_[:, 0:1], None, ALU.mult), reads=[xs_, rs_], writes=[xn_])

            def prep_tr(q, tt):
                kx = q * 4 + tt
                xn_ = xn[kx % 2]
                xg = xnT[q % 2]
                for c in range(8):
                    P.op("pe", _tr(pT[:, c * 128:(c + 1) * 128], xn_[:, c * 128:(c + 1) * 128], ident[:]),
                         reads=[xn_, ident], **({"writes": [pT]} if c == 0 else {"pwrites": [pT]}))
                P.op("act", _acp(xg[:, :, tt * 128:(tt + 1) * 128], pT[:].rearrange("p (c t) -> p c t", c=8)),
                     reads=[pT], **({"writes": [xg]} if tt == 0 else {"pwrites": [xg]}))

            def fm_item(q, idx):
                b, g = groups[q]
                xg = xnT[q % 2]
                (col0, M, dst, dT_, row0, gc) = FM[idx]
                kf = cn["kf"]
                cn["kf"] += 1
                pf = (pb[0], pb[1], pb[3])[kf % 3]
                for c in range(8):
                    P.op("pe", _mm(pf[0:M, :], W[:, c, col0:col0 + M], xg[:, c, :], c == 0, c == 7),
                         reads=[W, xg], **({"writes": [pf]} if c == 0 else {"pwrites": [pf]}))
                fs = fst[kf % 3]
                if col0 == C_KI:
                    M = 64
                if gc is not None:
                    sq_, sd2, rs2, pS = sqb[kf % 2], sdb[kf % 2], rsb[kf % 2], pb[2]
                    P.op("act", _act(sq_[:], pf[:], ACTF.Square), reads=[pf], writes=[sq_])
                yield
                if gc is not None:
                    P.op("pe", _mm(pS[:], bones[:], sq_[:], True, True), reads=[bones, sq_], writes=[pS])
                    P.op("act", _act(sd2[:], pS[:], ACTF.Ln, bias=epst[:, 0:1]), reads=[pS, epst], writes=[sd2])
                    P.op("act", _act(rs2[:], sd2[:], ACTF.Exp, scale=-0.5), reads=[sd2], writes=[rs2])
                    P.op("dve", _stt(fs[:], pf[:], qkg[:, gc:gc + 1], rs2[:], ALU.mult, ALU.mult),
                         reads=[pf, qkg, rs2], writes=[fs])
                else:
                    P.op("dve", _cp(fs[0:M, :], pf[0:M, :]), reads=[pf], writes=[fs])
                P.dma("pool", _dma(dst[b, row0:row0 + M, g * 512:(g + 1) * 512], fs[0:M, :]),
                      reads=[fs], pwrites=[dT_])

            def tm_item(q, tt, j):
                b, g = groups[q]
                xg = xnT[q % 2]
                t = 4 * g + tt
                (col0, kind, dst, dT_) = TM[j]
                pt = pb[4 + cn["kt"] % 2]
                cn["kt"] += 1
                for c in range(8):
                    P.op("pe", _mm(pt[:], xg[:, c, tt * 128:(tt + 1) * 128], W[:, c, col0:col0 + 512], c == 0, c == 7),
                         reads=[W, xg], **({"writes": [pt]} if c == 0 else {"pwrites": [pt]}))
                if kind == "v":
                    vs_ = vst[cn["kv"] % 2]
                    cn["kv"] += 1
                    P.op("dve", _cp(vs_[:, :, 0:64], pt[:].rearrange("p (h d) -> p h d", h=8)),
                         reads=[pt], pwrites=[vs_])
                    P.dma("pool", _dma(dst[b, t * 128:(t + 1) * 128, :], vs_[:].rearrange("p h d -> p (h d)")),
                          reads=[vs_], pwrites=[dT_])
                else:
                    gs_ = gst_[cn["kg"] % 2]
                    cn["kg"] += 1
                    P.op("act", _act(gs_[:], pt[:], ACTF.Silu), reads=[pt], writes=[gs_])
                    P.dma("pool", _dma(dst[b, t * 128:(t + 1) * 128, :], gs_[:]), reads=[gs_], pwrites=[dT_])
                if j == 3:
                    pw = pb[6]
                    for c in range(8):
                        P.op("pe", _mm(pw[:, 0:4], xg[:, c, tt * 128:(tt + 1) * 128], W[:, c, C_WI:C_WI + 4], c == 0, c == 7),
                             reads=[W, xg], **({"writes": [pw]} if c == 0 else {"pwrites": [pw]}))
                    P.op("dve", _ts(wi_all[:, b * NT + t, :], pw[:, 0:4], WSCALE, None, ALU.mult),
                         reads=[pw], pwrites=[wi_all])

            for tt in range(4):
                prep_norm(0, tt)
                prep_tr(0, tt)
            nb_b31()
            nbi = 0
            per_g = (len(nb_list) + len(groups) - 1) // len(groups)
            pend = [None]
            for q in range(len(groups)):
                vit = [("t", tt * 4 + j) for tt in range(4) for j in (0, 2)]
                git = [("t", tt * 4 + j) for tt in range(4) for j in (1, 3)]
                items = []
                for fi in range(len(FM)):
                    items.append(("f", fi))
                    if fi < len(vit):
                        items.append(vit[fi])
                items += git
                for n_, (kind, v) in enumerate(items):
                    if q + 1 < len(groups):
                        if n_ >= 2 and (n_ - 2) % 8 == 0 and (n_ - 2) // 8 < 4:
                            prep_norm(q + 1, (n_ - 2) // 8)
                        if n_ >= 6 and (n_ - 6) % 8 == 0 and (n_ - 6) // 8 < 4:
                            prep_tr(q + 1, (n_ - 6) // 8)
                    if kind == "f":
                        gnew = fm_item(q, v)
                        next(gnew)
                        if pend[0] is not None:
                            for _ in pend[0]:
                                pass
                        pend[0] = gnew
                    else:
                        tm_item(q, v // 4, v % 4)
                    if (n_ == len(items) - 1 or (kind == "t" and v % 2 == 1)) and pend[0] is not None:
                        for _ in pend[0]:
                            pass
                        pend[0] = None
                    if n_ in (10, 20, 30)[:per_g + 1] and nbi < len(nb_list):
                        nb_tile(*nb_list[nbi])
                        nbi += 1
            while nbi < len(nb_list):
                nb_tile(*nb_list[nbi])
                nbi += 1
            P.barrier()
            P.flush()

        for b in range(NB):
            with contextlib.ExitStack() as st:
                sb = mk(st)
                KaT = sb("KaT_s", [128, 4, S], BF16)
                VA = sb("VA_s", [128, NT, 520], BF16)
                kiT2 = sb("kiT2", [128, S], BF16)
                scb = [sb(f"sc{j}", [128, S], F32) for j in range(2)]
                nm = [sb(f"nm{j}", [128, S], BF16) for j in range(4)]
                rbuf = [sb(f"rbuf{j}", [128, 512], F32) for j in range(3)]
                pbuf = [sb(f"pbuf{j}", [128, 512], BF16) for j in range(6)]
                plb = [pb[2], pb[3], pb[4], pT]
                Qi = [sb(f"Qi{j}", [128, 8, 128], BF16) for j in range(4)]
                qii = [sb(f"qii{j}", [128, 4, 128], BF16) for j in range(2)]
                Gi = [sb(f"Gi{j}", [128, 8, 64], BF16) for j in range(4)]
                pvs = [sb(f"pvs{j}", [128, 8, 65], F32) for j in range(2)]
                rden = sb("rden", [128, 8, 1], F32)
                t1 = sb("t1", [128, 8, 64], F32)
                mixed = [sb(f"mixed{j}", [128, 8, 64], BF16) for j in range(2)]
                cntb = [sb(f"cnt{j}", [128, 1], F32) for j in range(2)]
                negmb = [sb(f"negm{j}", [128, 1], I32) for j in range(2)]
                candb = [sb(f"cand{j}", [128, 1], I32) for j in range(2)]
                kbb = [sb(f"kb{j}", [128, 1], I32) for j in range(2)]
                P.dma("sp", _dma(KaT[:], KaT_d[b].rearrange("(c p) s -> p c s", p=128)), reads=[dKa], writes=[KaT])
                P.dma("sp", _dma(VA[:], Va_d[b].rearrange("(t p) f -> p t f", p=128)), reads=[dVa], writes=[VA])
                P.dma("sp", _dma(kiT2[0:64, :], kiT_d[b]), reads=[dki], writes=[kiT2])
                P.dma("sp", _dma(kiT2[64:128, :], kiT_d[b]), reads=[dki], pwrites=[kiT2])
                qa_v = QaT_d[b].rearrange("(c hh p) s -> hh p c s", hh=2, p=64)
                qi_v = qiT_d[b].rearrange("(c hh p) s -> hh p c s", hh=2, p=64)
                for z_ in Qi + qii:
                    P.op("pool", lambda e, z_=z_: e.memset(z_[:], 0.0), writes=[z_])
                cnt_sc = [0]
                cnt_l = [0]

                def scores(i):
                    n = 128 * (i + 1)
                    Q_, q_, G_ = Qi[i % 4], qii[i % 2], Gi[i % 4]
                    sc = scb[i % 2]
                    isl = slice(i * 128, (i + 1) * 128)
                    P.dma("sp", _dma(Q_[0:64, 0:8:2, :], qa_v[0][:, :, isl]), reads=[dQa], pwrites=[Q_])
                    P.dma("sp", _dma(Q_[64:128, 1:8:2, :], qa_v[1][:, :, isl]), reads=[dQa], pwrites=[Q_])
                    P.dma("sp", _dma(q_[0:64, 0:4:2, :], qi_v[0][:, :, isl]), reads=[dqi], pwrites=[q_])
                    P.dma("sp", _dma(q_[64:128, 1:4:2, :], qi_v[1][:, :, isl]), reads=[dqi], pwrites=[q_])
                    P.dma("sp", _dma(G_[:].rearrange("p h d -> p (h d)"), Ga_d[b, i * 128:(i + 1) * 128, :]), reads=[dGa], writes=[G_])
                    P.dma("sp", _dma(sc[:, :n], tb_d[:, :n]), writes=[sc])
                    wcol = b * NT + i
                    for kg in range((n + 511) // 512):
                        ncol = min(512, n - kg * 512)
                        cs = slice(kg * 512, kg * 512 + ncol)
                        for h in range(4):
                            pk = pb[cnt_sc[0] % 2]
                            rb = rbuf[cnt_sc[0] % 3]
                            cnt_sc[0] += 1
                            hh, hc = h % 2, h // 2
                            P.op("pe", _mm(pk[:, :ncol], q_[:, h, :], kiT2[:, cs], True, True),
                                 reads=[q_, kiT2], writes=[pk])
                            P.op("act", _act(rb[:, :ncol], pk[:, :ncol], ACTF.Relu), reads=[pk], writes=[rb])
                            P.op("dve", _stt(sc[:, cs], rb[:, :ncol], wi_all[:, wcol, h:h + 1], sc[:, cs], ALU.mult, ALU.add),
                                 reads=[rb, wi_all, sc], pwrites=[sc])
                        yield
                    dsl = slice(i * 128, (i + 1) * 128)
                    P.op("dve", _tt(sc[:, dsl], sc[:, dsl], caus[:], ALU.add), reads=[sc, caus], pwrites=[sc])

                def bisect(blocks):
                    def count(i, mode, s1, rd):
                        n = 128 * (i + 1)
                        sc, cnt, jk = scb[i % 2], cntb[i % 2], nm[i % 4]
                        if mode == "dve":
                            P.op("dve", _ts(jk[:, :n], sc[:, :n], s1, None, ALU.is_ge, ALU.add, accum=cnt[:, 0:1]),
                                 reads=[sc] + rd, writes=[jk, cnt])
                        else:
                            P.op("act", _act(jk[:, :n], sc[:, :n], ACTF.Sign, bias=s1, scale=-1.0, accum=cnt[:, 0:1]),
                                 reads=[sc] + rd, writes=[jk, cnt])

                    def thrc(i, mode):
                        n = 128 * (i + 1)
                        return (KTH, ALU.is_ge, ALU.is_lt) if mode == "dve" else (-(2.0 * TOPK - n - 1.5), ALU.is_le, ALU.is_gt)
                    for (i, mode) in blocks:
                        if mode == "dve":
                            count(i, mode, 0.0, [])
                        else:
                            count(i, mode, zerot[:, 0:1], [zerot])
                    for (i, mode) in blocks:
                        cnt, negm, cand = cntb[i % 2], negmb[i % 2], candb[i % 2]
                        c, opk, opn = thrc(i, mode)
                        P.op("dve", _ts(negm[:], cnt[:], c, -1.0, opn, ALU.mult), reads=[cnt], writes=[negm])
                        P.op("dve", _stt(cand[:], negm[:], bitc[:, 30:31], bitc[:, 29:30], ALU.bitwise_and, ALU.bitwise_xor),
                             reads=[negm, bitc], writes=[cand])
                    yield
                    for bit in range(29, -1, -1):
                        for (i, mode) in blocks:
                            cand = candb[i % 2]
                            count(i, mode, cand[:, 0:1].bitcast(F32), [cand])
                        for (i, mode) in blocks:
                            cnt, cand, kb = cntb[i % 2], candb[i % 2], kbb[i % 2]
                            c, opk, opn = thrc(i, mode)
                            P.op("dve", _ts(kb[:], cnt[:], c, float(2 ** bit), opk, ALU.mult), reads=[cnt], writes=[kb])
                            P.op("dve", _stt(cand[:], kb[:], dbc[:, bit:bit + 1], cand[:], ALU.bitwise_xor, ALU.bitwise_xor),
                                 reads=[kb, dbc, cand], writes=[cand])
                        yield
                    for (i, mode) in blocks:
                        n = 128 * (i + 1)
                        P.op("dve", _ts(nm[i % 4][:, :n], scb[i % 2][:, :n], candb[i % 2][:, 0:1].bitcast(F32), NEGBIG, ALU.is_lt, ALU.mult),
                             reads=[scb[i % 2], candb[i % 2]], writes=[nm[i % 4]])
                    yield

                def stageA(m):
                    blocks = [i for i in (2 * m, 2 * m + 1) if i < NT]
                    for i in blocks:
                        yield from scores(i)
                    modes = ["dve", "act"]
                    yield from bisect([(i, modes[k]) for k, i in enumerate(blocks)])

                def attend(i):
                    Q_, G_ = Qi[i % 4], Gi[i % 4]
                    nm_ = nm[i % 4]
                    pv_ = pvs[i % 2]
                    ngr = (i + 4) // 4
                    steps = [(h, g) for h in range(8) for g in range(ngr)]
                    base = cnt_l[0]
                    cnt_l[0] += len(steps)

                    def plv(k):
                        t_ = plb[(base + k) % 4]
                        return t_, (t_.t[:].bitcast(F32) if t_ is pT else t_.t[:])

                    def qk(k):
                        h, g = steps[k]
                        pl, plap = plv(k)
                        hp, hh = h // 2, h % 2
                        chunks = list(range(4 * g, min(4 * g + 4, i + 1)))
                        for kk, c in enumerate(chunks):
                            o = plap[:, kk * 128:(kk + 1) * 128]
                            near = c >= i - 1
                            P.op("pe", _mm(o, KaT[:, hp, c * 128:(c + 1) * 128], Q_[:, h, :], True, False),
                                 reads=[KaT, Q_], **({"writes": [pl]} if kk == 0 else {"pwrites": [pl]}))
                            P.op("pe", _mm(o, nm_[:, c * 128:(c + 1) * 128], ident[:], False, not near),
                                 reads=[nm_, ident], pwrites=[pl])
                            if near:
                                nbs = NBt[0][h][:, 0:128] if c == i else NBt[0][h][:, 128:256]
                                P.op("pe", _mm(o, ident[:], nbs, False, True), reads=[NBt[0][h], ident], pwrites=[pl])

                    def ex(k):
                        h, g = steps[k]
                        pl, plap = plv(k)
                        pbf = pbuf[(base + k) % 6]
                        ncol = 128 * (min(4 * g + 4, i + 1) - 4 * g)
                        P.op("act", _act(pbf[:, :ncol], plap[:, :ncol], ACTF.Exp, bias=b31[:, h:h + 1], scale=0.125),
                             reads=[pl, b31], writes=[pbf])

                    def pv(k):
                        h, g = steps[k]
                        pbf = pbuf[(base + k) % 6]
                        pp = pb[5 + h % 2]
                        chunks = list(range(4 * g, min(4 * g + 4, i + 1)))
                        for kk, c in enumerate(chunks):
                            P.op("pe", _mm(pp[:, 0:65], pbf[:, kk * 128:(kk + 1) * 128], VA[:, c, h * 65:(h + 1) * 65], c == 0, c == i),
                                 reads=[pbf, VA], **({"writes": [pp]} if c == 0 else {"pwrites": [pp]}))
                        if g == ngr - 1:
                            P.op("act", _acp(pv_[:, h, :], pp[:, 0:65]), reads=[pp],
                                 **({"writes": [pv_]} if h == 0 else {"pwrites": [pv_]}))
                    qk(0)
                    if len(steps) > 1:
                        qk(1)
                    for k in range(len(steps)):
                        if k + 2 < len(steps):
                            qk(k + 2)
                        ex(k)
                        pv(k)
                        yield
                    mx = mixed[i % 2]
                    P.op("dve", lambda e: e.reciprocal(rden[:], pv_[:, :, 64:65]), reads=[pv_], writes=[rden])
                    P.op("dve", _tt(t1[:], pv_[:, :, 0:64], rden[:].to_broadcast([128, 8, 64]), ALU.mult), reads=[pv_, rden], writes=[t1])
                    P.op("dve", _tt(mx[:], t1[:], G_[:], ALU.mult), reads=[t1, G_], writes=[mx])
                    P.dma("pool", _dma(MixA_d[b, i * 128:(i + 1) * 128, :], mx[:].rearrange("p h d -> p (h d)")),
                          reads=[mx], pwrites=[dMix])
                    yield

                def stageB(m):
                    for i in (2 * m, 2 * m + 1):
                        if i < NT:
                            yield from attend(i)

                NP_ = (NT + 1) // 2

                def unitsA(m):
                    u = 0
                    for i in (2 * m, 2 * m + 1):
                        if i < NT:
                            u += (128 * (i + 1) + 511) // 512
                    return u + 32

                def unitsB(m):
                    u = 0
                    for i in (2 * m, 2 * m + 1):
                        if i < NT:
                            u += 8 * ((i + 4) // 4) + 1
                    return u
                order = list(range(NP_ - 1, -1, -1))
                for _ in stageA(order[0]):
                    pass
                for oi, m in enumerate(order):
                    mn = order[oi + 1] if oi + 1 < NP_ else None
                    ga = stageA(mn) if mn is not None else iter(())
                    gbb = stageB(m)
                    ua = unitsA(mn) if mn is not None else 0
                    ub = unitsB(m)
                    da = db = 0
                    a_alive, b_alive = ua > 0, True
                    while a_alive or b_alive:
                        if a_alive and (not b_alive or da * ub <= db * ua):
                            try:
                                next(ga)
                                da += 1
                            except StopIteration:
                                a_alive = False
                        else:
                            try:
                                next(gbb)
                                db += 1
                            except StopIteration:
                                b_alive = False
                P.barrier()
                P.flush()

            with contextlib.ExitStack() as st:
                sb = mk(st)
                QbT = sb("QbT_s", [128, 8, S], BF16)
                KbT = sb("KbT_s", [128, 4, S], BF16)
                vt = [sb(f"vt{j}", [128, 520], BF16) for j in range(3)]
                pbuf = [sb(f"pbufb{j}", [128, 512], BF16) for j in range(5)]
                plbB = [pb[0], pb[1], pb[2], pT]
                res = [sb(f"res{j}", [128, 520], F32) for j in range(2)]
                P.op("pool", lambda e: e.memset(QbT[:], 0.0), writes=[QbT])
                qb_v = QbT_d[b].rearrange("(c hh p) s -> hh p c s", hh=2, p=64)
                P.dma("sp", _dma(QbT[0:64, 0:8:2, :], qb_v[0]), reads=[dQb], pwrites=[QbT])
                P.dma("sp", _dma(QbT[64:128, 1:8:2, :], qb_v[1]), reads=[dQb], pwrites=[QbT])
                P.dma("sp", _dma(KbT[:], KbT_d[b].rearrange("(c p) s -> p c s", p=128)), reads=[dKb], writes=[KbT])
                tiles = []
                for p_, dil in enumerate(DILS):
                    nbk = S // (dil * 128)
                    for r in range(dil):
                        for c in range(nbk):
                            tiles.append((p_, dil, r, c))
                steps = [(ti, hp) for ti in range(len(tiles)) for hp in range(4)]

                def qsl(dil, r, c):
                    return slice(r + 128 * c * dil, r + 128 * c * dil + 127 * dil + 1, dil)

                def qkB(k):
                    ti, hp = steps[k]
                    p_, dil, r, c = tiles[ti]
                    qs = qsl(dil, r, c)
                    if hp == 0:
                        v_ = vt[ti % 3]
                        P.dma("sp", _dma(v_[:], Vb_d[b, qs, :]), reads=[dVb], writes=[v_])
                    pl = plbB[k % 4]
                    plap = pl.t[:].bitcast(F32) if pl is pT else pl.t[:]
                    firstw = True
                    for hh in range(2):
                        h = 2 * hp + hh
                        bs = hh * 256
                        srcs = [(0, qs)] + ([(128, qsl(dil, r, c - 1))] if c >= 1 else [])
                        for off, ks in srcs:
                            o = plap[:, bs + off:bs + off + 128]
                            P.op("pe", _mm(o, KbT[:, hp, ks], QbT[:, h, qs], True, True),
                                 reads=[KbT, QbT], **({"writes": [pl]} if firstw else {"pwrites": [pl]}))
                            firstw = False

                def exB(k):
                    ti, hp = steps[k]
                    p_, dil, r, c = tiles[ti]
                    pl = plbB[k % 4]
                    plap = pl.t[:].bitcast(F32) if pl is pT else pl.t[:]
                    pbf = pbuf[k % 5]
                    eb = EBt[p_][hp]
                    if c >= 1:
                        P.op("act", _act(pbf[:], plap, ACTF.Exp, scale=0.125), reads=[pl], writes=[pbf])
                        P.op("dve", _tt(pbf[:], pbf[:], eb[:], ALU.mult), reads=[pbf, eb], writes=[pbf])
                    else:
                        v3 = lambda t_: t_[:].rearrange("p (a b) -> p a b", a=2)[:, :, 0:128]
                        P.op("act", _act(v3(pbf), plap.rearrange("p (a b) -> p a b", a=2)[:, :, 0:128], ACTF.Exp, scale=0.125), reads=[pl], writes=[pbf])
                        P.op("dve", _tt(v3(pbf), v3(pbf), v3(eb), ALU.mult), reads=[pbf, eb], writes=[pbf])

                def pvB(k):
                    ti, hp = steps[k]
                    p_, dil, r, c = tiles[ti]
                    pbf = pbuf[k % 5]
                    v_ = vt[ti % 3]
                    vp = vt[(ti - 1) % 3]
                    for hh in range(2):
                        h = 2 * hp + hh
                        bs = hh * 256
                        pp = pb[3 + 2 * (ti % 2) + h // 4]
                        col = (h % 4) * 65
                        P.op("pe", _mm(pp[:, col:col + 65], pbf[:, bs:bs + 128], v_[:, h * 65:(h + 1) * 65], True, c == 0),
                             reads=[pbf, v_], **({"writes": [pp]} if h % 4 == 0 else {"pwrites": [pp]}))
                        if c >= 1:
                            P.op("pe", _mm(pp[:, col:col + 65], pbf[:, bs + 128:bs + 256], vp[:, h * 65:(h + 1) * 65], False, True),
                                 reads=[pbf, vp], pwrites=[pp])
                    if hp == 3:
                        rs_ = res[ti % 2]
                        P.op("act", _acp(rs_[:, 0:260], pb[3 + 2 * (ti % 2)][:, 0:260]), reads=[pb[3 + 2 * (ti % 2)]], writes=[rs_])
                        P.op("dve", _cp(rs_[:, 260:520], pb[4 + 2 * (ti % 2)][:, 0:260]), reads=[pb[4 + 2 * (ti % 2)]], pwrites=[rs_])
                        P.dma("pool", _dma(Bres_d[b, p_, qsl(dil, r, c), :], rs_[:]), reads=[rs_], pwrites=[dBres])
                nst = len(steps)
                for k0 in range(min(3, nst)):
                    qkB(k0)
                exB(0)
                for k in range(nst):
                    if k + 3 < nst:
                        qkB(k + 3)
                    if k + 1 < nst:
                        exB(k + 1)
                    pvB(k)
                P.barrier()
                P.flush()

            with contextlib.ExitStack() as st:
                sb = mk(st)
                xs = [sb(f"xf{j}", [128, 1024], F32) for j in range(3)]
                mix = [sb(f"mix{j}", [128, 1024], BF16) for j in range(3)]
                rr = [[sb(f"rr{j}_{q}", [128, 8, 65], F32) for q in range(3)] for j in range(3)]
                gb = [sb(f"gb{j}", [128, 8, 64], BF16) for j in range(3)]
                ssum = [sb(f"ssum{j}", [128, 8, 65], F32) for j in range(2)]
                rden = [sb(f"rdenf{j}", [128, 8, 1], F32) for j in range(2)]
                t1 = [sb(f"t1f{j}", [128, 8, 64], F32) for j in range(2)]
                mixT = [sb(f"mixT{j}", [128, 1024], BF16) for j in range(2)]
                ot = [sb(f"ot{j}", [128, 1024], F32) for j in range(2)]

                def FL(t):
                    j = t % 3
                    rows = slice(t * 128, (t + 1) * 128)
                    P.dma("sp", _dma(xs[j][:], x_d[b, rows, :]), writes=[xs[j]])
                    P.dma("sp", _dma(mix[j][:, 0:512], MixA_d[b, rows, :]), reads=[dMix], writes=[mix[j]])
                    for q in range(3):
                        P.dma("sp", _dma(rr[j][q][:].rearrange("p h d -> p (h d)"), Bres_d[b, q, rows, :]), reads=[dBres], writes=[rr[j][q]])
                    P.dma("sp", _dma(gb[j][:].rearrange("p h d -> p (h d)"), Gb_d[b, rows, :]), reads=[dGb], writes=[gb[j]])

                def F1(t):
                    j = t % 3
                    sm, rd, tt1 = ssum[t % 2], rden[t % 2], t1[t % 2]
                    P.op("dve", _tt(sm[:], rr[j][0][:], rr[j][1][:], ALU.add), reads=[rr[j][0], rr[j][1]], writes=[sm])
                    P.op("dve", _tt(sm[:], sm[:], rr[j][2][:], ALU.add), reads=[sm, rr[j][2]], writes=[sm])
                    P.op("dve", lambda e: e.reciprocal(rd[:], sm[:, :, 64:65]), reads=[sm], writes=[rd])
                    P.op("dve", _tt(tt1[:], sm[:, :, 0:64], rd[:].to_broadcast([128, 8, 64]), ALU.mult), reads=[sm, rd], writes=[tt1])
                    P.op("dve", _tt(mix[j][:, 512:1024].rearrange("p (h d) -> p h d", h=8), tt1[:], gb[j][:], ALU.mult),
                         reads=[tt1, gb[j]], pwrites=[mix[j]])

                def F2(t):
                    j = t % 3
                    j2 = t % 2
                    rows = slice(t * 128, (t + 1) * 128)
                    for c in range(8):
                        P.op("pe", _tr(pT[:, c * 128:(c + 1) * 128], mix[j][:, c * 128:(c + 1) * 128], ident[:]),
                             reads=[mix[j], ident], **({"writes": [pT]} if c == 0 else {"pwrites": [pT]}))
                    P.op("act", _acp(mixT[j2][:], pT[:]), reads=[pT], writes=[mixT[j2]])
                    for half in range(2):
                        po = pb[2 * j2 + half]
                        for c in range(8):
                            P.op("pe", _mm(po[:], mixT[j2][:, c * 128:(c + 1) * 128], Wo[:, c, half * 512:(half + 1) * 512], c == 0, c == 7),
                                 reads=[mixT[j2], Wo], **({"writes": [po]} if c == 0 else {"pwrites": [po]}))
                        P.op("dve", _tt(ot[j2][:, half * 512:(half + 1) * 512], po[:], xs[j][:, half * 512:(half + 1) * 512], ALU.add),
                             reads=[po, xs[j]], **({"writes": [ot[j2]]} if half == 0 else {"pwrites": [ot[j2]]}))
                    tok = P.dma("pool", _dma(out_d[b, rows, :], ot[j2][:]), reads=[ot[j2]])
                    out_tokens[tok[0]] = max(out_tokens.get(tok[0], 0), tok[1])
                FL(0)
                if NT > 1:
                    FL(1)
                F1(0)
                for t in range(NT):
                    if t + 2 < NT:
                        FL(t + 2)
                    if t + 1 < NT:
                        F1(t + 1)
                    F2(t)
                P.barrier()
                P.flush(final_tokens=out_tokens if b == NB - 1 else None)
    return nc


def _rel_bucket(d):
    d = np.maximum(np.asarray(d, np.int64), 0)
    df = np.maximum(d, 1).astype(np.float32)
    large = 16 + (np.log(df / np.float32(16)) / np.float32(math.log(128 / 16)) * np.float32(16)).astype(np.int32)
    large = np.minimum(large, 31)
    return np.where(d < 16, d, large).astype(np.int64)


def host_consts(S):
    bf = ml_dtypes.bfloat16
    c = {}
    c["ident"] = np.eye(128, dtype=np.float32).astype(bf)
    bo = np.zeros((128, 128), np.float32)
    bo[:64, :64] = 1.0 / 64
    bo[64:, 64:] = 1.0 / 64
    c["bones"] = bo.astype(bf)
    q = np.arange(128)[:, None]
    s = np.arange(128)[None, :]
    c["caus"] = np.where(s <= q, 0.0, -4.0).astype(np.float32)
    c["tb"] = np.tile((-(np.arange(S, dtype=np.float64) + 1) * 2.0 ** -100).astype(np.float32)[None], (128, 1))
    oh = np.zeros((32, 4 * 383), np.float32)
    neg = np.zeros((128, 3 * 383), np.float32)
    xs = np.arange(383)
    d = xs - 127
    bk = _rel_bucket(d)
    for x_ in range(383):
        if d[x_] >= 0:
            oh[bk[x_], x_] = 1.0
    for p_, dil in enumerate(DILS):
        valid = (d >= 0) & (d <= 128)
        bkp = _rel_bucket(d * dil)
        for x_ in range(383):
            if valid[x_]:
                oh[bkp[x_], (1 + p_) * 383 + x_] = 1.0
            else:
                neg[:, p_ * 383 + x_] = 8.0 * NEGBIG
    c["oh"] = oh
    c["negc"] = neg
    o31 = np.zeros((32, 128), np.float32)
    o31[31, :] = 1.0
    c["oh31"] = o31
    bc = np.zeros((128, 32), np.int32)
    for b in range(30):
        bc[:, b] = 1 << b
    bc[:, 30] = np.int32(-1073741825)
    c["bitc"] = bc
    db = np.zeros((128, 32), np.int32)
    db[:, 0] = 1
    for b in range(1, 30):
        db[:, b] = (1 << b) ^ (1 << (b - 1))
    c["dbc"] = db
    return c


def host_inputs(S, x_c, norm_gain, w_in, w_out, rel_bias, q_norm_a, k_norm_a, q_norm_b, k_norm_b):
    m = dict(host_consts(S))
    m["x"] = np.ascontiguousarray(x_c, dtype=np.float32)
    m["w_in"] = np.ascontiguousarray(w_in[0], dtype=np.float32)
    m["w_out"] = np.ascontiguousarray(w_out[0], dtype=np.float32)
    m["gain8"] = np.ascontiguousarray(np.asarray(norm_gain[0], np.float32).reshape(8, 128).T)
    m["relb"] = np.ascontiguousarray(rel_bias, dtype=np.float32)
    m["qkg"] = np.ascontiguousarray(np.stack(
        [np.tile(np.asarray(g[0], np.float32), 2) for g in (q_norm_a, k_norm_a, q_norm_b, k_norm_b)], axis=1))
    return m


_NC_CACHE = {}


def kernel(x, norm_gain, w_in, w_out, rel_bias, q_norm_a, k_norm_a, q_norm_b, k_norm_b):
    x = np.asarray(x)
    B, S, D = x.shape
    n = 8
    NB = B // n
    key = (S, NB)
    if key not in _NC_CACHE:
        _NC_CACHE[key] = build(S, NB)
    nc = _NC_CACHE[key]
    args = [np.asarray(a) for a in (norm_gain, w_in, w_out, rel_bias, q_norm_a, k_norm_a, q_norm_b, k_norm_b)]
    in_maps = [host_inputs(S, x[k * NB:(k + 1) * NB], *args) for k in range(n)]
    res = run_bass_kernel_spmd(nc, in_maps, core_ids=list(range(n)))
    return np.concatenate([r["out"] for r in res.results], axis=0).astype(np.float32)
```
